# Optimizing a Trainium2 kernel written in Bass

```python
import jax, jax.numpy as jnp
from jax import lax
import numpy as np

D_MODEL = 2048
BATCH = 4
SEQ = 4096
DEPTH = 1

N_MEM = 256
EPS = 1e-6
CHUNK = 64

GLA_HEADS = 4
GLA_DK = 128
GLA_DV = 256
GLA_K = GLA_HEADS * GLA_DK
GLA_V = GLA_HEADS * GLA_DV
GLA_RANK = 16
GLA_TAU = 16.0

MLSTM_HEADS = 4
MLSTM_DH = 256
MLSTM_W = MLSTM_HEADS * MLSTM_DH
CONV_WIDTH = 5

IN_SIZES = (GLA_K, GLA_K, GLA_V, GLA_V, GLA_RANK, GLA_RANK,
            MLSTM_W, MLSTM_W, 4 * MLSTM_HEADS, D_MODEL, D_MODEL)
IN_TOTAL = sum(IN_SIZES)

XATTN_HEADS = 4
XATTN_DH = D_MODEL // XATTN_HEADS

N_GROUPS = 4
EXPERTS_PER_GROUP = 8
N_EXPERTS = N_GROUPS * EXPERTS_PER_GROUP
TOP_K_IN_GROUP = 2
D_EXPERT = 512
MOE_BLOCK = 128

kernel_name = 'hybrid_gla_mlstm_xattn_hmoe_encoder'


def rmsnorm(x, g):
    xf = x.astype(jnp.float32)
    y = xf * lax.rsqrt(jnp.mean(xf * xf, axis=-1, keepdims=True) + EPS)
    return (y * g.astype(jnp.float32)).astype(x.dtype)


def _flip(t):
    return jnp.flip(t, axis=1)


def _chunk(t):
    b, s, h, d = t.shape
    return t.reshape(b, s // CHUNK, CHUNK, h, d).transpose(1, 0, 3, 2, 4)


def _unchunk(t):
    n, b, h, c, d = t.shape
    return t.transpose(1, 0, 3, 2, 4).reshape(b, n * c, h, d)


def _chunk_gate(t):
    b, s, h = t.shape
    return t.reshape(b, s // CHUNK, CHUNK, h).transpose(1, 0, 3, 2)


def gla_scan(q, k, v, log_a):
    bsz, seq, nh, dk = q.shape
    dv = v.shape[-1]
    qc, kc, vc = _chunk(q), _chunk(k), _chunk(v)
    bc = jnp.cumsum(_chunk(log_a), axis=3)
    mask = jnp.tril(jnp.ones((CHUNK, CHUNK), bool))[:, :, None]

    def step(state, xs):
        q_, k_, v_, b_ = xs
        inter = jnp.einsum('bhtd,bhde->bhte', q_ * jnp.exp(b_), state)
        diff = jnp.where(mask, b_[:, :, :, None, :] - b_[:, :, None, :, :], -jnp.inf)
        scores = jnp.einsum('bhtd,bhsd,bhtsd->bhts', q_, k_, jnp.exp(diff))
        intra = jnp.einsum('bhts,bhse->bhte', scores, v_)
        b_last = b_[:, :, -1:, :]
        k_dec = k_ * jnp.exp(b_last - b_)
        new_state = (jnp.exp(b_last[:, :, 0, :, None]) * state
                     + jnp.einsum('bhsd,bhse->bhde', k_dec, v_))
        return new_state, inter + intra

    s0 = jnp.zeros((bsz, nh, dk, dv), jnp.float32)
    _, out = lax.scan(step, s0, (qc, kc, vc, bc))
    return _unchunk(out)


def mlstm_scan(q, k, v, i_pre, f_pre):
    bsz, seq, nh, dh = q.shape
    qc, kc, vc = _chunk(q), _chunk(k), _chunk(v)
    ic = _chunk_gate(i_pre)
    bc = jnp.cumsum(_chunk_gate(jax.nn.log_sigmoid(f_pre)), axis=-1)
    mask = jnp.tril(jnp.ones((CHUNK, CHUNK), bool))

    def step(carry, xs):
        c_st, n_st, m_st = carry
        q_, k_, v_, i_, b_ = xs
        dmat = jnp.where(mask, b_[..., :, None] - b_[..., None, :] + i_[..., None, :], -jnp.inf)
        inter_log = b_ + m_st[..., None]
        m_t = jnp.maximum(inter_log, jnp.max(dmat, axis=-1))
        w_intra = jnp.exp(dmat - m_t[..., None])
        w_inter = jnp.exp(inter_log - m_t)
        s = jnp.einsum('bhtd,bhsd->bhts', q_, k_) * w_intra
        num = (w_inter[..., None] * jnp.einsum('bhtd,bhde->bhte', q_, c_st)
               + jnp.einsum('bhts,bhse->bhte', s, v_))
        den = w_inter * jnp.einsum('bhtd,bhd->bht', q_, n_st) + jnp.sum(s, axis=-1)
        h = num / jnp.maximum(jnp.abs(den), jnp.exp(-m_t))[..., None]
        b_last = b_[..., -1]
        upd_log = b_last[..., None] - b_ + i_
        m_new = jnp.maximum(b_last + m_st, jnp.max(upd_log, axis=-1))
        w_old = jnp.exp(b_last + m_st - m_new)
        kw = k_ * jnp.exp(upd_log - m_new[..., None])[..., None]
        c_new = w_old[..., None, None] * c_st + jnp.einsum('bhsd,bhse->bhde', kw, v_)
        n_new = w_old[..., None] * n_st + jnp.sum(kw, axis=-2)
        return (c_new, n_new, m_new), h

    init = (jnp.zeros((bsz, nh, dh, dh), jnp.float32),
            jnp.zeros((bsz, nh, dh), jnp.float32),
            jnp.zeros((bsz, nh), jnp.float32))
    _, out = lax.scan(step, init, (qc, kc, vc, ic, bc))
    return _unchunk(out)


def hybrid_mixer(xn, w_in, gla_w_lr_f, gla_b_lr_f, gla_w_lr_b, gla_b_lr_b, gla_norm,
                 conv_w, conv_b, m_wq, m_wk, m_wv, m_gate_bias, m_norm,
                 w_branch_a, w_branch_b, w_mix_out):
    bsz, seq, _ = xn.shape
    dt = xn.dtype
    proj = xn @ w_in
    offsets = [int(o) for o in np.cumsum(IN_SIZES)[:-1]]
    (gq, gk, gv, gg, lr_f, lr_b, mx, mz, mgates, gate_a, gate_b) = jnp.split(proj, offsets, axis=-1)

    f32 = jnp.float32
    q = gq.astype(f32).reshape(bsz, seq, GLA_HEADS, GLA_DK) * (GLA_DK ** -0.5)
    k = gk.astype(f32).reshape(bsz, seq, GLA_HEADS, GLA_DK)
    v = gv.astype(f32).reshape(bsz, seq, GLA_HEADS, GLA_DV)
    la_f = (jax.nn.log_sigmoid((lr_f @ gla_w_lr_f + gla_b_lr_f).astype(f32)) / GLA_TAU
            ).reshape(bsz, seq, GLA_HEADS, GLA_DK)
    la_b = (jax.nn.log_sigmoid((lr_b @ gla_w_lr_b + gla_b_lr_b).astype(f32)) / GLA_TAU
            ).reshape(bsz, seq, GLA_HEADS, GLA_DK)
    o_a = gla_scan(q, k, v, la_f) + _flip(gla_scan(_flip(q), _flip(k), _flip(v), _flip(la_b)))
    y_a = rmsnorm(o_a, gla_norm.reshape(GLA_HEADS, GLA_DV)).reshape(bsz, seq, GLA_V)
    y_a = (y_a * jax.nn.silu(gg.astype(f32))).astype(dt)

    xc = lax.conv_general_dilated(mx, conv_w, window_strides=(1,),
                                  padding=[(CONV_WIDTH // 2, CONV_WIDTH // 2)],
                                  dimension_numbers=('NWC', 'WIO', 'NWC'),
                                  feature_group_count=MLSTM_W) + conv_b
    xc = jax.nn.silu(xc.astype(f32)).reshape(bsz, seq, MLSTM_HEADS, MLSTM_DH)
    qm = jnp.einsum('bshd,hde->bshe', xc, m_wq.astype(f32))
    km = jnp.einsum('bshd,hde->bshe', xc, m_wk.astype(f32)) * (MLSTM_DH ** -0.5)
    vm = jnp.einsum('bshd,hde->bshe', mx.astype(f32).reshape(bsz, seq, MLSTM_HEADS, MLSTM_DH),
                    m_wv.astype(f32))
    gates = mgates.astype(f32).reshape(bsz, seq, 4, MLSTM_HEADS) + m_gate_bias.astype(f32)
    h_f = mlstm_scan(qm, km, vm, gates[:, :, 0], gates[:, :, 1])
    h_b = _flip(mlstm_scan(_flip(qm), _flip(km), _flip(vm),
                           _flip(gates[:, :, 2]), _flip(gates[:, :, 3])))
    y_b = rmsnorm(h_f + h_b, m_norm.reshape(MLSTM_HEADS, MLSTM_DH)).reshape(bsz, seq, MLSTM_W)
    y_b = (y_b * jax.nn.sigmoid(mz.astype(f32))).astype(dt)

    merged = (jax.nn.sigmoid(gate_a) * (y_a @ w_branch_a)
              + jax.nn.sigmoid(gate_b) * (y_b @ w_branch_b))
    return merged @ w_mix_out


def cross_attention(xn, mem_n, w_xq, w_xkv, w_xo):
    bsz, seq, _ = xn.shape
    q = (xn @ w_xq).reshape(bsz, seq, XATTN_HEADS, XATTN_DH)
    k, v = jnp.split(mem_n @ w_xkv, 2, axis=-1)
    k = k.reshape(bsz, N_MEM, XATTN_HEADS, XATTN_DH)
    v = v.reshape(bsz, N_MEM, XATTN_HEADS, XATTN_DH)
    scores = jnp.einsum('bshd,bmhd->bhsm', q, k).astype(jnp.float32) * (XATTN_DH ** -0.5)
    probs = jax.nn.softmax(scores, axis=-1).astype(xn.dtype)
    out = jnp.einsum('bhsm,bmhd->bshd', probs, v).reshape(bsz, seq, D_MODEL)
    return out @ w_xo


def hier_moe(xn, w_group, b_group, w_router, b_router, w_gate, w_up, w_down):
    bsz, seq, d = xn.shape
    n_tok = bsz * seq
    xt = xn.reshape(n_tok, d)
    g_prob = jax.nn.softmax((xt @ w_group).astype(jnp.float32) + b_group.astype(jnp.float32), axis=-1)
    g_sel = jnp.argmax(g_prob, axis=-1)
    p_g = jnp.take_along_axis(g_prob, g_sel[:, None], axis=-1)[:, 0]
    e_all = jnp.einsum('td,gde->tge', xt, w_router).astype(jnp.float32) + b_router.astype(jnp.float32)
    e_logits = jnp.take_along_axis(e_all, g_sel[:, None, None], axis=1)[:, 0]
    e_prob = jax.nn.softmax(e_logits, axis=-1)
    top_p, top_i = lax.top_k(e_prob, TOP_K_IN_GROUP)
    weights = p_g[:, None] * top_p / jnp.sum(top_p, axis=-1, keepdims=True)
    expert = g_sel[:, None] * EXPERTS_PER_GROUP + top_i

    n_asg = n_tok * TOP_K_IN_GROUP
    flat_e = expert.reshape(-1)
    flat_w = weights.reshape(-1)
    flat_t = jnp.repeat(jnp.arange(n_tok, dtype=jnp.int32), TOP_K_IN_GROUP)
    order = jnp.argsort(flat_e)
    se = flat_e[order]
    counts = jnp.bincount(flat_e, length=N_EXPERTS)
    padded = ((counts + MOE_BLOCK - 1) // MOE_BLOCK) * MOE_BLOCK
    starts = jnp.cumsum(counts) - counts
    pends = jnp.cumsum(padded)
    pstarts = pends - padded
    dest = pstarts[se] + (jnp.arange(n_asg) - starts[se])
    n_rows = n_asg + N_EXPERTS * MOE_BLOCK
    n_blocks = n_rows // MOE_BLOCK
    row_tok = jnp.full((n_rows,), n_tok, jnp.int32).at[dest].set(flat_t[order])
    row_w = jnp.zeros((n_rows,), jnp.float32).at[dest].set(flat_w[order])
    blk_e = jnp.clip(jnp.searchsorted(pends, jnp.arange(n_blocks) * MOE_BLOCK, side='right'),
                     0, N_EXPERTS - 1)
    x_pad = jnp.concatenate([xt, jnp.zeros((1, d), xt.dtype)], axis=0)
    x_rows = x_pad[row_tok].reshape(n_blocks, MOE_BLOCK, d)

    def expert_block(args):
        xb, e = args
        return (jax.nn.silu(xb @ w_gate[e]) * (xb @ w_up[e])) @ w_down[e]

    y_rows = lax.map(expert_block, (x_rows, blk_e)).reshape(n_rows, d)
    y_rows = (y_rows.astype(jnp.float32) * row_w[:, None]).astype(xt.dtype)
    y = jax.ops.segment_sum(y_rows, row_tok, num_segments=n_tok + 1)[:n_tok]
    return y.reshape(bsz, seq, d)


def setup_inputs(seed: int = 0) -> dict:
    key = jax.random.key(seed)
    ks = jax.random.split(key, 40)
    f32 = jnp.float32
    L = DEPTH

    def nrm(k, shape, scale):
        return jax.random.normal(k, shape, f32) * scale

    def gain(k, shape):
        return 1.0 + 0.02 * jax.random.normal(k, shape, f32)

    fb = jnp.linspace(3.0, 6.0, MLSTM_HEADS, dtype=f32)
    zb = jnp.zeros((MLSTM_HEADS,), f32)
    gate_base = jnp.stack([zb, fb, zb, fb])[None]
    return {
        'x': jax.random.normal(ks[0], (BATCH, SEQ, D_MODEL), f32),
        'mem': jax.random.normal(ks[1], (BATCH, N_MEM, D_MODEL), f32),
        'norm_mix': gain(ks[2], (L, D_MODEL)),
        'w_in': nrm(ks[3], (L, D_MODEL, IN_TOTAL), D_MODEL ** -0.5),
        'gla_w_lr_f': nrm(ks[4], (L, GLA_RANK, GLA_K), GLA_RANK ** -0.5),
        'gla_b_lr_f': nrm(ks[5], (L, GLA_K), 0.1),
        'gla_w_lr_b': nrm(ks[6], (L, GLA_RANK, GLA_K), GLA_RANK ** -0.5),
        'gla_b_lr_b': nrm(ks[7], (L, GLA_K), 0.1),
        'gla_norm': gain(ks[8], (L, GLA_V)),
        'conv_w': nrm(ks[9], (L, CONV_WIDTH, 1, MLSTM_W), CONV_WIDTH ** -0.5),
        'conv_b': nrm(ks[10], (L, MLSTM_W), 0.02),
        'm_wq': nrm(ks[11], (L, MLSTM_HEADS, MLSTM_DH, MLSTM_DH), MLSTM_DH ** -0.5),
        'm_wk': nrm(ks[12], (L, MLSTM_HEADS, MLSTM_DH, MLSTM_DH), MLSTM_DH ** -0.5),
        'm_wv': nrm(ks[13], (L, MLSTM_HEADS, MLSTM_DH, MLSTM_DH), MLSTM_DH ** -0.5),
        'm_gate_bias': gate_base + nrm(ks[14], (L, 4, MLSTM_HEADS), 0.1),
        'm_norm': gain(ks[15], (L, MLSTM_W)),
        'w_branch_a': nrm(ks[16], (L, GLA_V, D_MODEL), GLA_V ** -0.5),
        'w_branch_b': nrm(ks[17], (L, MLSTM_W, D_MODEL), MLSTM_W ** -0.5),
        'w_mix_out': nrm(ks[18], (L, D_MODEL, D_MODEL), D_MODEL ** -0.5),
        'norm_xattn': gain(ks[19], (L, D_MODEL)),
        'norm_mem': gain(ks[20], (L, D_MODEL)),
        'w_xq': nrm(ks[21], (L, D_MODEL, D_MODEL), D_MODEL ** -0.5),
        'w_xkv': nrm(ks[22], (L, D_MODEL, 2 * D_MODEL), D_MODEL ** -0.5),
        'w_xo': nrm(ks[23], (L, D_MODEL, D_MODEL), D_MODEL ** -0.5),
        'norm_ffn': gain(ks[24], (L, D_MODEL)),
        'w_group': nrm(ks[25], (L, D_MODEL, N_GROUPS), D_MODEL ** -0.5),
        'b_group': nrm(ks[26], (L, N_GROUPS), 0.01),
        'w_router': nrm(ks[27], (L, N_GROUPS, D_MODEL, EXPERTS_PER_GROUP), D_MODEL ** -0.5),
        'b_router': nrm(ks[28], (L, N_GROUPS, EXPERTS_PER_GROUP), 0.01),
        'w_gate': nrm(ks[29], (L, N_EXPERTS, D_MODEL, D_EXPERT), D_MODEL ** -0.5),
        'w_up': nrm(ks[30], (L, N_EXPERTS, D_MODEL, D_EXPERT), D_MODEL ** -0.5),
        'w_down': nrm(ks[31], (L, N_EXPERTS, D_EXPERT, D_MODEL), D_EXPERT ** -0.5),
        'norm_final': gain(ks[32], (D_MODEL,)),
    }


def reference(x, mem, norm_mix, w_in, gla_w_lr_f, gla_b_lr_f, gla_w_lr_b, gla_b_lr_b, gla_norm,
              conv_w, conv_b, m_wq, m_wk, m_wv, m_gate_bias, m_norm,
              w_branch_a, w_branch_b, w_mix_out,
              norm_xattn, norm_mem, w_xq, w_xkv, w_xo,
              norm_ffn, w_group, b_group, w_router, b_router, w_gate, w_up, w_down,
              norm_final):
    h = x
    for l in range(DEPTH):
        h = h + hybrid_mixer(rmsnorm(h, norm_mix[l]), w_in[l],
                             gla_w_lr_f[l], gla_b_lr_f[l], gla_w_lr_b[l], gla_b_lr_b[l], gla_norm[l],
                             conv_w[l], conv_b[l], m_wq[l], m_wk[l], m_wv[l], m_gate_bias[l], m_norm[l],
                             w_branch_a[l], w_branch_b[l], w_mix_out[l])
        h = h + cross_attention(rmsnorm(h, norm_xattn[l]), rmsnorm(mem, norm_mem[l]),
                                w_xq[l], w_xkv[l], w_xo[l])
        h = h + hier_moe(rmsnorm(h, norm_ffn[l]), w_group[l], b_group[l], w_router[l], b_router[l],
                         w_gate[l], w_up[l], w_down[l])
    return rmsnorm(h, norm_final)
```

```python
import numpy as np
from contextlib import ExitStack
import concourse.bass as bass
import concourse.mybir as mybir
from concourse.bass_utils import run_bass_kernel_spmd

F32 = mybir.dt.float32
BF16 = mybir.dt.bfloat16
I32 = mybir.dt.int32
U32 = mybir.dt.uint32
F32R = mybir.dt.float32r
AF = mybir.ActivationFunctionType
ALU = mybir.AluOpType
AX = mybir.AxisListType


class Sched:
    def __init__(self, nc, es, n_dma=6):
        self.nc = nc
        self.es = es
        self.eng = {'pe': nc.tensor, 'dve': nc.vector, 'act': nc.scalar,
                    'pool': nc.gpsimd, 'sp': nc.sync}
        self.sem = {}
        self.cnt = {}
        for k in ('pe', 'dve', 'act', 'pool'):
            self.sem[k] = es.enter_context(nc.semaphore("s_" + k))
            self.cnt[k] = 0
        self.known = {k: {} for k in self.eng}
        self.dsem = {}
        self.dpos = {}
        for q in ('sp', 'pool', 'act'):
            n = {'sp': 16, 'pool': 12, 'act': 8}[q]
            self.dsem[q] = [[es.enter_context(nc.semaphore("d_%s%d" % (q, i))), 0] for i in range(n)]
            self.dpos[q] = 0
        self.dsem['bg'] = [[es.enter_context(nc.semaphore("d_bg%d" % i)), 0] for i in range(4)]
        self.dpos['bg'] = 0
        self.res_w = {}
        self.res_r = {}
        self.persist = {}
        self.semname = {}
        self.n_inst = 0

    def sb(self, name, shape, dt):
        return self.es.enter_context(self.nc.sbuf_tensor(name, shape, dt))

    def ps(self, name, shape, dt):
        return self.es.enter_context(self.nc.psum_tensor(name, shape, dt))

    def _deps(self, reads, writes):
        evs = []
        for r in reads:
            w = self.res_w.get(r)
            if w is not None:
                evs.append(w)
            w = self.persist.get(r)
            if w is not None:
                evs.append(w)
        for w_ in writes:
            w = self.res_w.get(w_)
            if w is not None:
                evs.append(w)
            evs.extend(self.res_r.get(w_, {}).values())
        return evs

    def _wait(self, e, evs, skip_own=False):
        kn = self.known[e]
        best = {}
        for (sem, val, key) in evs:
            if skip_own and key == e:
                continue
            if kn.get(key, 0) >= val:
                continue
            if key not in best or best[key][1] < val:
                best[key] = (sem, val)
        for key, (sem, val) in best.items():
            self.eng[e].wait_ge(sem, val)
            kn[key] = val
            self.n_inst += 1

    def _record(self, ev, reads, writes):
        for r in reads:
            d = self.res_r.setdefault(r, {})
            old = d.get(ev[2])
            if old is None or old[1] < ev[1]:
                d[ev[2]] = ev
        for w_ in writes:
            self.res_w[w_] = ev
            self.res_r[w_] = {}

    def wait_for(self, e, reads=(), writes=()):
        self._wait(e, self._deps(reads, writes))

    def op(self, e, fn, reads=(), writes=()):
        self._wait(e, self._deps(reads, writes), skip_own=(e == 'pe'))
        inst = fn(self.eng[e])
        self.cnt[e] += 1
        inst.then_inc(self.sem[e], 1)
        ev = (self.sem[e], self.cnt[e], e)
        self._record(ev, reads, writes)
        self.n_inst += 1
        return inst

    def _dma_slot(self, q):
        slots = self.dsem[q]
        i = self.dpos[q]
        self.dpos[q] = (i + 1) % len(slots)
        return i, slots[i]

    def dma(self, q, out, in_, reads=(), writes=(), **kw):
        i, slot = self._dma_slot(q)
        key = "d_%s%d" % (q, i)
        evs = self._deps(reads, writes)
        if slot[1] > 0:
            evs.append((slot[0], slot[1], key))
        self._wait(q, evs)
        inst = self.eng[q].dma_start(out=out, in_=in_, **kw)
        slot[1] += 16
        inst.then_inc(slot[0], 16)
        ev = (slot[0], slot[1], key)
        self._record(ev, reads, writes)
        self.n_inst += 1
        return inst

    def dma_bg(self, out, in_, reads=(), writes=()):
        i, slot = self._dma_slot('bg')
        key = "d_bg%d" % i
        evs = self._deps(reads, [])
        if slot[1] > 0:
            evs.append((slot[0], slot[1], key))
        self._wait('pool', evs)
        inst = self.eng['pool'].dma_start(out=out, in_=in_)
        slot[1] += 16
        inst.then_inc(slot[0], 16)
        ev = (slot[0], slot[1], key)
        for w_ in writes:
            self.persist[w_] = ev
        self.n_inst += 1
        return inst

    def dma_indirect(self, q, out, out_offset, in_, in_offset, reads=(), writes=(), **kw):
        assert q == 'pool'
        i, slot = self._dma_slot(q)
        key = "d_%s%d" % (q, i)
        evs = self._deps(reads, writes)
        if slot[1] > 0:
            evs.append((slot[0], slot[1], key))
        self._wait(q, evs)
        inst = self.eng[q].indirect_dma_start(out=out, out_offset=out_offset, in_=in_,
                                              in_offset=in_offset, **kw)
        slot[1] += 16
        inst.then_inc(slot[0], 16)
        ev = (slot[0], slot[1], key)
        self._record(ev, reads, writes)
        self.n_inst += 1
        return inst

    def barrier(self):
        evs = []
        for k in ('pe', 'dve', 'act', 'pool'):
            if self.cnt[k] > 0:
                evs.append((self.sem[k], self.cnt[k], k))
        for q in self.dsem:
            if q == 'bg':
                continue
            for i, slot in enumerate(self.dsem[q]):
                if slot[1] > 0:
                    evs.append((slot[0], slot[1], "d_%s%d" % (q, i)))
        for e in ('pe', 'dve', 'act', 'pool', 'sp'):
            self._wait(e, evs)
        self.res_w = {}
        self.res_r = {}

    def finish(self, outs):
        self._wait('sp', self._deps(outs, []))
        evs = []
        for k in ('pe', 'dve', 'act', 'pool'):
            if self.cnt[k] > 0:
                evs.append((self.sem[k], self.cnt[k], k))
        for q in self.dsem:
            for i, slot in enumerate(self.dsem[q]):
                if slot[1] > 0:
                    evs.append((slot[0], slot[1], "d_%s%d" % (q, i)))
        self._wait('sp', evs)


D = 2048
SEQ = 4096
NB = 4
OWN = 2048
NT_OWN = 16
NT_ALL = 32
EPS = 1e-6
KT = 16
NEXP = 32
NBLK = 64
NROWS = NBLK * 128
import math
LN_QS = math.log(128 ** -0.5)
LN_KS = math.log(256 ** -0.5)

DEBUG = False
WCONV_POST = (('gz', 4, 16, 512), ('ba', 8, 8, 256), ('bb', 8, 8, 256), ('gab', 16, 16, 256),
              ('mix', 4, 16, 512), ('xq', 8, 16, 256), ('xo', 4, 16, 512))
WCONV_A = (('q', 1, 16, 512), ('k', 1, 16, 512), ('v', 2, 16, 512), ('mx', 2, 16, 512))
WSRC = {'xkv': 'w_xkv', 'gz': 'w_gz', 'ba': 'w_ba', 'bb': 'w_bb', 'gab': 'w_gab', 'mix': 'w_mix',
        'xq': 'w_xq', 'xo': 'w_xo', 'q': 'w_q', 'k': 'w_k', 'v': 'w_v', 'mx': 'w_mx'}
STOP_AFTER = 99


class Ctx:
    pass


def build_program(debug=False, stop_after=99):
    nc = bass.Bass("TRN2", target_bir_lowering=False)
    C = Ctx()
    C.nc = nc

    def din(name, shape, dt=F32):
        return nc.dram_tensor(name, list(shape), dt, kind="ExternalInput").ap()

    def dscr(name, shape, dt=F32, dbg=False):
        kind = "ExternalOutput" if (debug and dbg) else "Internal"
        return nc.dram_tensor(name, list(shape), dt, kind=kind).ap()

    I = {}
    I['x'] = din('x', [SEQ, D])
    I['mem'] = din('mem', [256, D])
    for nm in ('g_mix', 'g_xattn', 'g_mem', 'g_ffn', 'g_final'):
        I[nm] = din(nm, [128, D])
    I['g_gla'] = din('g_gla', [128, 1024])
    I['g_m'] = din('g_m', [128, 1024])
    I['w_q'] = din('w_q', [D, 512])
    I['w_k'] = din('w_k', [D, 512])
    I['w_v'] = din('w_v', [D, 1024])
    I['w_mx'] = din('w_mx', [D, 1024])
    I['w_sm'] = din('w_sm', [D, 48])
    I['w_gz'] = din('w_gz', [D, 2048])
    I['w_gab'] = din('w_gab', [D, 4096])
    I['wlr1'] = din('wlr1', [2, 17, 512])
    I['rf1'] = din('rf1', [2, 17, 512])
    I['ri1'] = din('ri1', [2, 17, 512])
    I['conv_w'] = din('conv_w', [128, 8, 5])
    I['conv_b'] = din('conv_b', [128, 8])
    I['m_wq'] = din('m_wq', [4, 256, 256])
    I['m_wk'] = din('m_wk', [4, 256, 256])
    I['m_wv'] = din('m_wv', [4, 256, 256])
    I['w_ba'] = din('w_ba', [1024, D])
    I['w_bb'] = din('w_bb', [1024, D])
    I['w_mix'] = din('w_mix', [D, D])
    I['w_xq'] = din('w_xq', [D, D])
    I['w_xkv'] = din('w_xkv', [D, 2 * D])
    I['w_xo'] = din('w_xo', [D, D])
    I['w_rt'] = din('w_rt', [D, 36])
    I['b_rt'] = din('b_rt', [128, 36])
    I['w_gate'] = din('w_gate', [NEXP, D, 512])
    I['w_up'] = din('w_up', [NEXP, D, 512])
    I['w_down'] = din('w_down', [NEXP, 512, D])
    I['c_ident'] = din('c_ident', [128, 128])
    I['c_negi'] = din('c_negi', [128, 128])
    I['c_ones'] = din('c_ones', [128, 128])
    I['c_mincl'] = din('c_mincl', [2, 128, 128])
    I['c_ustr'] = din('c_ustr', [2, 128, 128])
    I['c_iota32'] = din('c_iota32', [128, 32])
    I['c_brow'] = din('c_brow', [32, 64])
    I['c_jrow'] = din('c_jrow', [128, 4])
    I['c_zero'] = din('c_zero', [128, D])
    out = nc.dram_tensor('out', [OWN, D], F32, kind="ExternalOutput").ap()

    Z = {}
    Z['mxT'] = dscr('z_mxT', [8, 128, SEQ + 4], F32, dbg=True)
    Z['mgT'] = dscr('z_mgT', [16, SEQ], F32, dbg=True)
    for f, (hk, dv) in enumerate(((4 * 128, 1024), (4 * 256, 4 * 257))):
        Z['QT%d' % f] = dscr('z_QT%d' % f, [2, NT_OWN, 128, hk], BF16, dbg=True)
        Z['KT%d' % f] = dscr('z_KT%d' % f, [2, NT_OWN, 128, hk], BF16, dbg=True)
        Z['KD%d' % f] = dscr('z_KD%d' % f, [2, NT_ALL, 128, hk], BF16, dbg=True)
        Z['V%d' % f] = dscr('z_V%d' % f, [NT_ALL, 128, dv], BF16, dbg=True)
    Z['YN'] = dscr('z_YN', [NT_OWN, 128, 2048], F32, dbg=True)
    for nm, nblk, nk, ncol in WCONV_POST + WCONV_A:
        Z['W_' + nm] = dscr('z_W_' + nm, [nblk, 128, nk, ncol], BF16)
    Z['H2'] = dscr('z_H2', [NT_OWN, 128, D], F32, dbg=True)
    Z['XN3'] = dscr('z_XN3', [NT_OWN, 128, D], F32, dbg=True)
    Z['XR'] = dscr('z_XR', [NROWS, D], F32)
    Z['YR'] = dscr('z_YR', [NROWS, D], F32)
    Z['RT'] = dscr('z_RT', [NT_OWN, 128, 8], F32, dbg=True)
    Z['BE'] = dscr('z_BE', [1, 64], F32, dbg=True)

    with ExitStack() as es:
        S = Sched(nc, es)
        C.S = S
        pb = [S.ps("pb%d" % i, [128, 512], F32) for i in range(8)]
        C.pbi = 0

        def nb():
            i = C.pbi
            C.pbi = (i + 1) % 8
            return pb[i], "pb%d" % i
        C.nb = nb

        ident = S.sb("ident", [128, 128], F32)
        negi = S.sb("negi", [128, 128], F32)
        ones = S.sb("ones", [128, 128], F32)
        mincl = S.sb("mincl", [128, 2, 128], F32)
        ustr = S.sb("ustr", [128, 2, 128], F32)
        maskb = S.sb("maskb", [128, 2, 128], F32)
        S.dma('sp', ident[:], I['c_ident'], writes=['ident'])
        S.dma('sp', negi[:], I['c_negi'], writes=['negi'])
        S.dma('sp', ones[:], I['c_ones'], writes=['ones'])
        for d in range(2):
            S.dma('sp', mincl[:, d, :], I['c_mincl'][d], writes=['mincl'])
            S.dma('sp', ustr[:, d, :], I['c_ustr'][d], writes=['ustr'])
            S.dma('sp', maskb[:, d, :], I['c_mincl'][d], writes=['maskb'])
        elast = [S.sb("elast%d" % f, [128, 2, NT_ALL, 4], F32) for f in range(2)]
        C.RT = S.sb("RT", [128, NT_OWN, 8], F32)
        S.op('dve', lambda e: e.memset(C.RT[:], 0.0), writes=['RT'])
        C.__dict__.update(dict(ident=ident, negi=negi, ones=ones, mincl=mincl, ustr=ustr,
                               maskb=maskb, elast=elast, pb=pb, I=I, Z=Z, out=out))

        stage_A(C, stop_after)
        if stop_after >= 2:
            S.barrier()
            stage_B(C)
        if stop_after >= 3:
            S.barrier()
            stage_scan(C, 0)
            S.barrier()
            stage_scan(C, 1)
        if stop_after >= 4:
            S.barrier()
            stage_post(C)
        if stop_after >= 5:
            S.barrier()
            stage_moe(C)
        outs = ['out'] if stop_after >= 5 else []
        S.barrier()
        S.finish(outs)
    return nc


def mm(C, out, lhsT, rhs, start, stop, reads, writes):
    C.S.op('pe', lambda e: e.matmul(out, lhsT=lhsT, rhs=rhs, start=start, stop=stop),
           reads=reads, writes=writes)


def act(C, out, in_, func, reads, writes, **kw):
    C.S.op('act', lambda e: e.activation(out=out, in_=in_, func=func, **kw), reads=reads, writes=writes)


def tt(C, out, in0, in1, op, reads, writes, eng='dve'):
    C.S.op(eng, lambda e: e.tensor_tensor(out=out, in0=in0, in1=in1, op=op), reads=reads, writes=writes)


def stt(C, out, in0, scalar, in1, op0, op1, reads, writes):
    C.S.op('dve', lambda e: e.scalar_tensor_tensor(out=out, in0=in0, scalar=scalar, in1=in1, op0=op0, op1=op1),
           reads=reads, writes=writes)


def ts(C, out, in0, s1, s2, op0, op1, reads, writes):
    if op1 is None:
        C.S.op('dve', lambda e: e.tensor_scalar(out=out, in0=in0, scalar1=s1, scalar2=None, op0=op0),
               reads=reads, writes=writes)
    else:
        C.S.op('dve', lambda e: e.tensor_scalar(out=out, in0=in0, scalar1=s1, scalar2=s2, op0=op0, op1=op1),
               reads=reads, writes=writes)


def cp(C, eng, out, in_, reads, writes):
    if eng == 'act':
        C.S.op('act', lambda e: e.copy(out=out, in_=in_), reads=reads, writes=writes)
    else:
        C.S.op(eng, lambda e: e.tensor_copy(out=out, in_=in_), reads=reads, writes=writes)


def rms_rstd(C, x_ap, x_res, width, junk, junk_res, ss, rstd, tag):
    act(C, junk, x_ap, AF.Square, [x_res], [junk_res, tag + 'ss'], accum_out=ss)
    act(C, ss, ss, AF.Sqrt, [tag + 'ss'], [tag + 'ss'], scale=1.0 / width, bias=EPS)
    C.S.op('dve', lambda e: e.reciprocal(out=rstd, in_=ss), reads=[tag + 'ss'], writes=[tag + 'rstd'])


def norm_T(C, x_ap, x_res, gain, gain_res, xn, xn_res, W, outT, outT_res, tile_sl, out_dt_copy='act'):
    W_ = W
    rms_rstd(C, x_ap, x_res, D, W_['junk'][:], 'junk', W_['ss'][:], W_['rstd'][:], 'nt_')
    stt(C, xn, x_ap, W_['rstd'][:, 0:1], gain, ALU.mult, ALU.mult, [x_res, 'nt_rstd', gain_res], [xn_res])
    transpose_to(C, xn, xn_res, KT, outT, outT_res, tile_sl)


def transpose_to(C, src, src_res, nk, outT, outT_res, tile_sl, interleave=False):
    srcv = src.rearrange("r (p k) -> r k p", k=nk) if interleave else None
    for k4 in range(0, nk, 4):
        p, pn = C.nb()
        n = min(4, nk - k4)
        for j in range(n):
            k = k4 + j
            sin = srcv[:, k, :] if interleave else src[:, k * 128:(k + 1) * 128]
            C.S.op('pe', lambda e: e.transpose(p[:, j * 128:(j + 1) * 128], sin, C.ident[:]),
                   reads=[src_res, 'ident'], writes=[pn])
        eng = 'act' if (k4 // 4) % 2 == 0 else 'dve'
        cp(C, eng, outT[:, k4:k4 + n, tile_sl], p[:, 0:n * 128].rearrange("p (k t) -> p k t", k=n),
           [pn], [outT_res])


def load_w_block(C, wbuf, wres, src_ap, nk, ncol):
    C.S.dma('pool', wbuf[:, 0:nk, 0:ncol], src_ap.rearrange("(k p) c -> p k c", p=128), writes=[wres])


def convert_weights(C, spec, bg=False):
    for item in spec:
        nm, nblk, ncol = item[0], item[1], item[3]
        src = C.I[WSRC[nm]]
        for blk in range(nblk):
            sap = src[:, blk * ncol:(blk + 1) * ncol].rearrange("(k p) c -> p k c", p=128)
            if bg:
                C.S.dma_bg(C.Z['W_' + nm][blk], sap, writes=[('Wc', nm, blk)])
            else:
                C.S.dma('pool', C.Z['W_' + nm][blk], sap, writes=[('Wc', nm, blk)])


def load_w_tiled(C, wbuf, wres, nm, blk, nk, ncol):
    C.S.dma('pool', wbuf[:, 0:nk, 0:ncol], C.Z['W_' + nm][blk], reads=[('Wc', nm, blk)], writes=[wres])


def gate_prep(C, f, c, own, dirs, lhs_fn, lhs_res_fn, rw, ri, W, ktok, ktok_res, qT, kT, tsl, filler=None):
    S = C.S
    Z = C.Z
    gs = (1.0 / 16.0) if f == 0 else 1.0
    lnk = 0.0 if f == 0 else LN_KS
    lnq = LN_QS if f == 0 else 0.0
    N = {d: dict(zip(('sp', 'isb', 'dec', 'kd', 'eb', 'enb', 'qt', 'kt'),
                     ['%s%d%s' % (n_, d, ('_%d' % (c % 2)) if n_ in ('kd', 'qt', 'kt') else '') for n_ in ('sp', 'isb', 'dec', 'kd', 'eb', 'enb', 'qt', 'kt')])) for d in dirs}
    for d in dirs:
        z, zn = C.nb()
        mm(C, z[:, :], lhs_fn(d), rw[0:17, d, :], True, True, [lhs_res_fn(d), 'rw%d' % f], [zn])
        sp = W['sp'][d]
        act(C, sp[:], z[:, :], AF.Exp, [zn], [N[d]['sp']], scale=-1.0)
        act(C, sp[:], sp[:], AF.Ln, [N[d]['sp']], [N[d]['sp']], bias=1.0, scale=1.0)
        if f == 1:
            zi, zin = C.nb()
            mm(C, zi[:, :], lhs_fn(d), ri[0:17, d, :], True, True, [lhs_res_fn(d), 'ri'], [zin])
            cp(C, 'dve', W['isb'][d][:], zi[:, :], [zin], [N[d]['isb']])
    if filler is not None:
        filler()
    P2 = {}
    for d in dirs:
        sp = W['sp'][d]
        isb = W['isb'][d]
        SPN, ISN = N[d]['sp'], N[d]['isb']
        dp, dpn = C.nb()
        mm(C, dp[:, :], C.ustr[:, d, :], sp[:], True, f == 0, ['ustr', SPN], [dpn])
        if f == 1:
            mm(C, dp[:, :], C.negi[:], isb[:], False, True, ['negi', ISN], [dpn])
        cs, csn = C.nb()
        for h in range(4):
            mm(C, cs[:, h * 128:(h + 1) * 128], sp[:, h * 128:(h + 1) * 128], C.mincl[:, d, :], True, True,
               [SPN, 'mincl'], [csn])
        ci, cin = None, None
        if own and f == 1:
            ci, cin = C.nb()
            for h in range(4):
                mm(C, ci[:, h * 128:(h + 1) * 128], sp[:, h * 128:(h + 1) * 128], C.mincl[:, d, :], True, False,
                   [SPN, 'mincl'], [cin])
                mm(C, ci[:, h * 128:(h + 1) * 128], isb[:, h * 128:(h + 1) * 128], C.ident[:], False, True,
                   [ISN, 'ident'], [cin])
        P2[d] = (dp, dpn, cs, csn, ci, cin)
    for d in dirs:
        dp, dpn, cs, csn, ci, cin = P2[d]
        DCN, KDN, EBN, ENN, QTN, KTN = [N[d][k_] for k_ in ('dec', 'kd', 'eb', 'enb', 'qt', 'kt')]
        dec = W['dec'][d]
        act(C, dec[:], dp[:, :], AF.Exp, [dpn], [DCN], scale=-gs, bias=lnk)
        kd = W['kd%d' % f][d][c % 2]
        if f == 0:
            tt(C, kd[:], ktok[:], dec[:], ALU.mult, [ktok_res, DCN], [KDN])
        else:
            kv = ktok[:].rearrange("p (h k d) -> p h k d", h=4, k=2)
            kdv = kd[:].rearrange("p (h k d) -> p h k d", h=4, k=2)
            dv_ = dec[:].rearrange("p (h d) -> p h d", h=4)
            for k in range(2):
                tt(C, kdv[:, :, k, :], kv[:, :, k, :], dv_, ALU.mult, [ktok_res, DCN], [KDN])
        S.dma('sp', Z['KD%d' % f][d, c], kd[:], reads=[KDN], writes=[('KD', f, d, c)])
        col = 127 if d == 0 else 0
        csv = cs[:, :].rearrange("p (h t) -> p h t", h=4)
        act(C, C.elast[f][:, d, c, :], csv[:, :, col], AF.Exp, [csn], [('elast', f)], scale=-gs)
        if own:
            eb = W['eb'][d]
            enb = W['enb'][d]
            act(C, eb[:], cs[:, :], AF.Exp, [csn], [EBN], scale=-gs, bias=lnq)
            if f == 0:
                act(C, enb[:], cs[:, :], AF.Exp, [csn], [ENN], scale=gs)
            else:
                act(C, enb[:], ci[:, :], AF.Exp, [cin], [ENN], scale=1.0, bias=lnk)
            qt = W['qt%d' % f][d][c % 2]
            kt_ = W['kt%d' % f][d][c % 2]
            ebv = eb[:].rearrange("p (h t) -> p h t", h=4)
            enbv = enb[:].rearrange("p (h t) -> p h t", h=4)
            if f == 0:
                tt(C, qt[:].rearrange("p (h t) -> p h t", h=4), qT[:, :, tsl], ebv, ALU.mult, ['qT', EBN], [QTN])
                tt(C, kt_[:].rearrange("p (h t) -> p h t", h=4), kT[:, :, tsl], enbv, ALU.mult, ['kT', ENN], [KTN])
            else:
                qtv = qt[:].rearrange("p (h k t) -> p h k t", h=4, k=2)
                ktv = kt_[:].rearrange("p (h k t) -> p h k t", h=4, k=2)
                qTv = qT[:].rearrange("p (h k) t -> p h k t", h=4)
                kTv = kT[:].rearrange("p (h k) t -> p h k t", h=4)
                for k in range(2):
                    tt(C, qtv[:, :, k, :], qTv[:, :, k, tsl], ebv, ALU.mult, ['qT', EBN], [QTN])
                    tt(C, ktv[:, :, k, :], kTv[:, :, k, tsl], enbv, ALU.mult, ['kT', ENN], [KTN])
            S.dma('sp', Z['QT%d' % f][d, c], qt[:], reads=[QTN], writes=[('QT', f, d, c)])
            S.dma('sp', Z['KT%d' % f][d, c], kt_[:], reads=[KTN], writes=[('KT', f, d, c)])


def alloc_gate_work(C, es2, W, pfx=''):
    S = C.S

    def sbt(name, shape, dt):
        return es2.enter_context(C.nc.sbuf_tensor(pfx + name, shape, dt))
    for nm_ in ('sp', 'isb', 'dec', 'eb', 'enb'):
        W[nm_] = [sbt("g_%s%d" % (nm_, d), [128, 512], F32) for d in range(2)]
    return sbt


def stage_A(C, stop_after):
    S = C.S
    I = C.I
    Z = C.Z
    nc = C.nc
    with ExitStack() as es2:
        W = {}
        sbt = alloc_gate_work(C, es2, W)
        W['junk'] = sbt("a_junk", [128, D], BF16)
        W['ss'] = sbt("a_ss", [128, 1], F32)
        W['rstd'] = sbt("a_rstd", [128, 1], F32)
        gain = sbt("a_gain", [128, D], F32)
        xbuf = [sbt("a_x%d" % i, [128, D], F32) for i in range(2)]
        xn = sbt("a_xn", [128, D], F32)
        xnT = sbt("a_xnT", [128, KT, 512], BF16)
        wbuf = [sbt("a_w%d" % i, [128, KT, 512], BF16) for i in range(3)]
        wsm = sbt("a_wsm", [128, KT, 48], BF16)
        qT = sbt("a_qT", [128, 4, 512], F32)
        kT = sbt("a_kT", [128, 4, 512], F32)
        stg = [sbt("a_stg%d" % i, [128, 512], F32) for i in range(2)]
        ktok = sbt("a_ktok", [128, 512], F32)
        vtok = sbt("a_vtok", [128, 1024], BF16)
        lr1T = sbt("a_lr1T", [17, 2, 512], F32)
        mgs = sbt("a_mgs", [16, 512], F32)
        rw = sbt("a_rw", [17, 2, 512], F32)
        zt = sbt("a_zero", [128, 8, 2], F32)
        W['kd0'] = [[sbt("a_kd%d%d" % (d, i), [128, 512], BF16) for i in range(2)] for d in range(2)]
        W['qt0'] = [[sbt("a_qt%d%d" % (d, i), [128, 512], BF16) for i in range(2)] for d in range(2)]
        W['kt0'] = [[sbt("a_kt%d%d" % (d, i), [128, 512], BF16) for i in range(2)] for d in range(2)]

        S.dma('sp', gain[:], I['g_mix'], writes=['gain'])
        for d in range(2):
            S.dma('sp', rw[:, d, :], I['wlr1'][d], writes=['rw0'])
        S.op('dve', lambda e: e.memset(lr1T[:], 1.0), writes=['lr1T0', 'lr1T1'])
        S.op('dve', lambda e: e.memset(zt[:], 0.0), writes=['zt'])
        S.dma('sp', Z['mxT'][:, :, 0:2].rearrange("c p t -> p c t"), zt[:], reads=['zt'], writes=[('mxT', 'h0')])
        S.dma('sp', Z['mxT'][:, :, SEQ + 2:SEQ + 4].rearrange("c p t -> p c t"), zt[:], reads=['zt'], writes=[('mxT', 'h1')])
        load_w_block(C, wsm, 'wsm', I['w_sm'], KT, 48)
        convert_weights(C, WCONV_A)

        wi = [0]

        def next_w(nm, blk):
            i = wi[0]
            wi[0] = (i + 1) % 3
            load_w_tiled(C, wbuf[i], 'w%d' % i, nm, blk, KT, 512)
            return wbuf[i], 'w%d' % i

        si = [0]

        def next_stg():
            i = si[0]
            si[0] = (i + 1) % 2
            return stg[i], 'stg%d' % i

        def fm_block(w, wres, ncol_tiles, M, evac):
            for ct in range(ncol_tiles):
                p, pn = C.nb()
                for k in range(KT):
                    mm(C, p[0:M, :], w[:, k, ct * 128:ct * 128 + M], xnT[:, k, :], k == 0, k == KT - 1,
                       [wres, 'xnT'], [pn])
                evac(ct, p, pn)

        for g in range(8):
            own = g < 4
            t0 = g * 512
            for j in range(4):
                t = g * 4 + j
                xb = xbuf[t % 2]
                xr = 'x%d' % (t % 2)
                if not (g > 0 and j < 2):
                    S.dma('sp', xb[:], I['x'][t * 128:(t + 1) * 128, :], writes=[xr])
                norm_T(C, xb[:], xr, gain[:], 'gain', xn[:], 'xn', W, xnT, 'xnT', slice(j * 128, (j + 1) * 128))
            if g + 1 < 8:
                for j in range(2):
                    t = (g + 1) * 4 + j
                    S.dma('sp', xbuf[t % 2][:], I['x'][t * 128:(t + 1) * 128, :], writes=['x%d' % (t % 2)])
            for sidx in range(3):
                if sidx == 0 and not own:
                    continue
                p, pn = C.nb()
                for k in range(KT):
                    mm(C, p[0:16, :], wsm[:, k, sidx * 16:(sidx + 1) * 16], xnT[:, k, :], k == 0, k == KT - 1,
                       ['wsm', 'xnT'], [pn])
                if sidx < 2:
                    cp(C, 'act', lr1T[0:16, sidx, :], p[0:16, :], [pn], ['lr1T%d' % sidx])
                else:
                    cp(C, 'act', mgs[:], p[0:16, :], [pn], ['mgs'])
                    S.dma('sp', Z['mgT'][:, t0:t0 + 512], mgs[:], reads=['mgs'], writes=[('mgT', g)])
            if own:
                w, wr = next_w('q', 0)
                fm_block(w, wr, 4, 128, lambda ct, p, pn: cp(C, 'act', qT[:, ct, :], p[:, :], [pn], ['qT']))
            w, wr = next_w('k', 0)
            if own:
                fm_block(w, wr, 4, 128, lambda ct, p, pn: cp(C, 'dve', kT[:, ct, :], p[:, :], [pn], ['kT']))
            wk, wkr = w, wr
            wv_ = [next_w('v', vb) for vb in range(2)]
            for j in range(4):
                c = g * 4 + j
                tsl = slice(j * 128, (j + 1) * 128)
                p, pn = C.nb()
                for k in range(KT):
                    mm(C, p[:, :], xnT[:, k, tsl], wk[:, k, :], k == 0, k == KT - 1, ['xnT', wkr], [pn])
                cp(C, 'act', ktok[:], p[:, :], [pn], ['ktok'])

                def v_filler(j=j, c=c, tsl=tsl):
                    for vb in range(2):
                        w, wr = wv_[vb]
                        p, pn = C.nb()
                        for k in range(KT):
                            mm(C, p[:, :], xnT[:, k, tsl], w[:, k, :], k == 0, k == KT - 1, ['xnT', wr], [pn])
                        cp(C, 'act' if vb == 0 else 'dve', vtok[:, vb * 512:(vb + 1) * 512], p[:, :], [pn], [('vtok', vb)])
                        S.dma('sp', Z['V0'][c, :, vb * 512:(vb + 1) * 512], vtok[:, vb * 512:(vb + 1) * 512],
                              reads=[('vtok', vb)], writes=[('V0', c, vb)])
                gate_prep(C, 0, c, own, (0, 1) if own else (1,),
                          lambda d: lr1T[0:17, d, tsl], lambda d: 'lr1T%d' % d, rw, None, W,
                          ktok, 'ktok', qT, kT, tsl, filler=v_filler)
            for mb in range(2):
                w, wr = next_w('mx', mb)

                def ev_mx(ct, p, pn, mb=mb):
                    s_, sr = next_stg()
                    cp(C, 'act' if ct % 2 == 0 else 'dve', s_[:], p[:, :], [pn], [sr])
                    S.dma('sp', Z['mxT'][mb * 4 + ct, :, 2 + t0:2 + t0 + 512], s_[:], reads=[sr],
                          writes=[('mxT', g, mb, ct)])
                fm_block(w, wr, 4, 128, ev_mx)


def _consts():
    c = {}
    ii = np.arange(128)
    c['c_ident'] = np.eye(128, dtype=np.float32)
    c['c_negi'] = np.where(np.eye(128) > 0, np.float32(-1.0), np.float32(0.0)).astype(np.float32)
    c['c_ones'] = np.ones((128, 128), np.float32)
    s = ii[:, None]
    t = ii[None, :]
    c['c_mincl'] = np.stack([(s <= t), (s >= t)]).astype(np.float32)
    c['c_ustr'] = np.stack([(s > t), (s < t)]).astype(np.float32)
    c['c_iota32'] = np.tile(np.arange(32, dtype=np.float32)[None, :], (128, 1))
    c['c_brow'] = np.tile(np.arange(64, dtype=np.float32)[None, :] * np.float32(128.0), (32, 1)).astype(np.float32)
    c['c_jrow'] = np.tile(np.arange(4, dtype=np.float32)[None, :], (128, 1))
    c['c_zero'] = np.zeros((128, D), np.float32)
    return c


def _rep(v, n=128):
    return np.ascontiguousarray(np.broadcast_to(np.asarray(v, np.float32).reshape(1, -1), (n, v.size)))


def _prep_shared(inp):
    w_in = np.asarray(inp['w_in'][0], np.float32)
    offs = np.cumsum([0, 512, 512, 1024, 1024, 16, 16, 1024, 1024, 16, 2048, 2048])
    gq, gk, gv, gg, lrf, lrb, mx, mz, mg, ga, gb = [w_in[:, offs[i]:offs[i + 1]] for i in range(11)]
    base = {}
    base['w_q'] = np.ascontiguousarray(gq)
    base['w_k'] = np.ascontiguousarray(gk)
    base['w_v'] = np.ascontiguousarray(gv)
    base['w_mx'] = np.ascontiguousarray(mx)
    base['w_gz'] = np.ascontiguousarray(np.concatenate([gg, mz], axis=1))
    base['w_gab'] = np.ascontiguousarray(np.concatenate([ga, gb], axis=1))
    for nm, key in (('g_mix', 'norm_mix'), ('g_xattn', 'norm_xattn'), ('g_mem', 'norm_mem'),
                    ('g_ffn', 'norm_ffn'), ('g_gla', 'gla_norm'), ('g_m', 'm_norm')):
        base[nm] = _rep(np.asarray(inp[key][0], np.float32))
    base['g_final'] = _rep(np.asarray(inp['norm_final'], np.float32))
    for nm in ('m_wq', 'm_wk', 'm_wv'):
        base[nm] = np.ascontiguousarray(np.asarray(inp[nm][0], np.float32))
    base['w_ba'] = np.ascontiguousarray(inp['w_branch_a'][0])
    base['w_bb'] = np.ascontiguousarray(inp['w_branch_b'][0])
    base['w_mix'] = np.ascontiguousarray(inp['w_mix_out'][0])
    base['w_xq'] = np.ascontiguousarray(inp['w_xq'][0])
    base['w_xkv'] = np.ascontiguousarray(inp['w_xkv'][0])
    base['w_xo'] = np.ascontiguousarray(inp['w_xo'][0])
    wr = np.asarray(inp['w_router'][0], np.float32)
    base['w_rt'] = np.ascontiguousarray(np.concatenate([np.asarray(inp['w_group'][0], np.float32)] +
                                                       [wr[g] for g in range(4)], axis=1))
    base['b_rt'] = _rep(np.concatenate([np.asarray(inp['b_group'][0], np.float32),
                                        np.asarray(inp['b_router'][0], np.float32).reshape(-1)]))
    base['w_gate'] = np.ascontiguousarray(inp['w_gate'][0])
    base['w_up'] = np.ascontiguousarray(inp['w_up'][0])
    base['w_down'] = np.ascontiguousarray(inp['w_down'][0])
    base['conv_b'] = np.ascontiguousarray(np.asarray(inp['conv_b'][0], np.float32).reshape(8, 128).T)
    base.update(_consts())
    res = []
    cw = np.asarray(inp['conv_w'][0], np.float32)[:, 0, :]
    gbias = np.asarray(inp['m_gate_bias'][0], np.float32)
    wlr = [np.asarray(inp['gla_w_lr_f'][0], np.float32), np.asarray(inp['gla_w_lr_b'][0], np.float32)]
    blr = [np.asarray(inp['gla_b_lr_f'][0], np.float32), np.asarray(inp['gla_b_lr_b'][0], np.float32)]
    lrs = [lrf, lrb]
    for half in range(2):
        dct = dict(base)
        o = [0, 1] if half == 0 else [1, 0]
        mgl = np.concatenate([mg[:, 8 * o[0]:8 * o[0] + 8], mg[:, 8 * o[1]:8 * o[1] + 8]], axis=1)
        dct['w_sm'] = np.ascontiguousarray(np.concatenate([lrs[o[0]], lrs[o[1]], mgl], axis=1))
        wl = np.zeros((2, 17, 512), np.float32)
        rf = np.zeros((2, 17, 512), np.float32)
        ri = np.zeros((2, 17, 512), np.float32)
        for d in range(2):
            wl[d, 0:16] = wlr[o[d]]
            wl[d, 16] = blr[o[d]]
            for h in range(4):
                ri[d, 8 * d + h, h * 128:(h + 1) * 128] = 1.0
                ri[d, 16, h * 128:(h + 1) * 128] = gbias[2 * o[d], h]
                rf[d, 8 * d + 4 + h, h * 128:(h + 1) * 128] = 1.0
                rf[d, 16, h * 128:(h + 1) * 128] = gbias[2 * o[d] + 1, h]
        dct['wlr1'] = wl
        dct['rf1'] = rf
        dct['ri1'] = ri
        cwl = cw if half == 0 else cw[::-1]
        dct['conv_w'] = np.ascontiguousarray(cwl.T.reshape(8, 128, 5).transpose(1, 0, 2))
        res.append(dct)
    return res


def _in_maps(inp):
    shared = _prep_shared(inp)
    x = np.asarray(inp['x'], np.float32)
    mem = np.asarray(inp['mem'], np.float32)
    maps = []
    for core in range(8):
        b, half = core // 2, core % 2
        m = dict(shared[half])
        xs = x[b] if half == 0 else x[b][::-1]
        m['x'] = np.ascontiguousarray(xs)
        m['mem'] = np.ascontiguousarray(mem[b])
        maps.append(m)
    return maps


def kernel(**inputs):
    nc = build_program()
    maps = _in_maps(inputs)
    res = run_bass_kernel_spmd(nc, maps, core_ids=list(range(8)))
    out = np.zeros((NB, SEQ, D), np.float32)
    for core in range(8):
        b, half = core // 2, core % 2
        o = np.asarray(res.results[core]['out'], np.float32)
        if half == 0:
            out[b, 0:OWN] = o
        else:
            out[b, OWN:] = o[::-1]
    return out


def stage_B(C):
    S = C.S
    I = C.I
    Z = C.Z
    with ExitStack() as es2:
        W = {}
        sbt = alloc_gate_work(C, es2, W, 'B')
        mxg2 = [sbt("b_mxg%d" % i, [128, 8, 516], F32) for i in range(2)]
        xcT = sbt("b_xcT", [128, 8, 512], BF16)
        mxgb = sbt("b_mxgb", [128, 8, 516], BF16)
        dg = sbt("b_dg", [128, 8, 5, 128], BF16)
        cw = sbt("b_cw", [128, 8, 5], F32)
        cb = sbt("b_cb", [128, 8], F32)
        wq = sbt("b_wq", [128, 4, 2, 256], BF16)
        wk = sbt("b_wk", [128, 4, 2, 256], BF16)
        wv = sbt("b_wv", [128, 4, 2, 256], BF16)
        qT = sbt("b_qT", [128, 8, 512], F32)
        kT = sbt("b_kT", [128, 8, 512], F32)
        ktok = sbt("b_ktok", [128, 1024], F32)
        vp2 = [sbt("b_vp%d" % i, [128, 4, 257], BF16) for i in range(2)]
        mg1T2 = [sbt("b_mg1T%d" % i, [17, 512], F32) for i in range(2)]
        rf = sbt("b_rf", [17, 2, 512], F32)
        ri = sbt("b_ri", [17, 2, 512], F32)
        W['kd1'] = [[sbt("b_kd%d%d" % (d, i), [128, 1024], BF16) for i in range(2)] for d in range(2)]
        W['qt1'] = [[sbt("b_qt%d%d" % (d, i), [128, 1024], BF16) for i in range(2)] for d in range(2)]
        W['kt1'] = [[sbt("b_kt%d%d" % (d, i), [128, 1024], BF16) for i in range(2)] for d in range(2)]

        S.dma('sp', cw[:], I['conv_w'], writes=['cw'])
        S.dma('sp', cb[:], I['conv_b'], writes=['cb'])
        for d in range(2):
            S.dma('sp', rf[:, d, :], I['rf1'][d], writes=['rw1'])
            S.dma('sp', ri[:, d, :], I['ri1'][d], writes=['ri'])
        for wt, nm in ((wq, 'm_wq'), (wk, 'm_wk'), (wv, 'm_wv')):
            for h in range(4):
                S.dma('pool', wt[:, h, :, :], I[nm][h].rearrange("(k p) e -> p k e", p=128), writes=['b_' + nm])
        for i in range(2):
            S.op('dve', lambda e: e.memset(mg1T2[i][:], 1.0), writes=['mg1T%d' % i])
            S.op('dve', lambda e: e.memset(vp2[i][:], 1.0), writes=['vp%d' % i])
        for ct in range(8):
            for j in range(5):
                ts(C, dg[:, ct, j, :], C.ident[:], cw[:, ct, j:j + 1], None, ALU.mult, None, ['ident', 'cw'], ['dg'])
        convert_weights(C, WCONV_POST, bg=True)
        for b in range(NBLK):
            S.dma_bg(Z['XR'][b * 128:(b + 1) * 128, :], I['c_zero'], writes=[('XRz', b)])

        def b_loads(g):
            t0 = g * 512
            S.dma('sp', mxg2[g % 2][:], Z['mxT'][:, :, t0:t0 + 516].rearrange("c p t -> p c t"), writes=['mxg%d' % (g % 2)])
            S.dma('sp', mg1T2[g % 2][0:16, :], Z['mgT'][:, t0:t0 + 512], writes=['mg1T%d' % (g % 2)])

        b_loads(0)
        for g in range(8):
            own = g < 4
            t0 = g * 512
            mxg, MXGN = mxg2[g % 2], 'mxg%d' % (g % 2)
            mg1T, MGN = mg1T2[g % 2], 'mg1T%d' % (g % 2)
            if g + 1 < 8:
                b_loads(g + 1)
            cp(C, 'act', mxgb[:], mxg[:], [MXGN], ['mxb'])
            for ct in range(8):
                p, pn = C.nb()
                for j in range(5):
                    mm(C, p[:, :], dg[:, ct, j, :], mxgb[:, ct, j:j + 512], j == 0, j == 4, ['dg', 'mxb'], [pn])
                act(C, xcT[:, ct, :], p[:, :], AF.Silu, [pn, 'cb'], ['xcT'], bias=cb[:, ct:ct + 1], scale=1.0)
            if own:
                for (wt, wn, dst, dn) in ((wq, 'b_m_wq', qT, 'qT'), (wk, 'b_m_wk', kT, 'kT')):
                    for h in range(4):
                        for et in range(2):
                            p, pn = C.nb()
                            for dt_ in range(2):
                                mm(C, p[:, :], wt[:, h, dt_, et * 128:(et + 1) * 128], xcT[:, 2 * h + dt_, :],
                                   dt_ == 0, dt_ == 1, [wn, 'xcT'], [pn])
                            cp(C, 'act' if et == 0 else 'dve', dst[:, h * 2 + et, :], p[:, :], [pn], [dn])
            for j in range(4):
                c = g * 4 + j
                tsl = slice(j * 128, (j + 1) * 128)
                vp, VPN = vp2[c % 2], 'vp%d' % (c % 2)
                for hp in range(2):
                    p, pn = C.nb()
                    p2, pn2 = C.nb()
                    for h2 in range(2):
                        h = hp * 2 + h2
                        for dt_ in range(2):
                            mm(C, p[:, h2 * 256:(h2 + 1) * 256], xcT[:, 2 * h + dt_, tsl], wk[:, h, dt_, :],
                               dt_ == 0, dt_ == 1, ['xcT', 'b_m_wk'], [pn])
                        for dt_ in range(2):
                            mm(C, p2[:, h2 * 256:(h2 + 1) * 256], mxgb[:, 2 * h + dt_, 2 + j * 128:2 + (j + 1) * 128], wv[:, h, dt_, :],
                               dt_ == 0, dt_ == 1, ['mxb', 'b_m_wv'], [pn2])
                    cp(C, 'act', ktok[:, hp * 512:(hp + 1) * 512], p[:, :], [pn], ['ktok'])
                    cp(C, 'dve', vp[:, hp * 2:hp * 2 + 2, 0:256], p2[:, :].rearrange("p (h e) -> p h e", h=2),
                       [pn2], [VPN])
                S.dma('sp', Z['V1'][c], vp[:].rearrange("p h e -> p (h e)"), reads=[VPN], writes=[('V1', c)])
                gate_prep(C, 1, c, own, (0, 1) if own else (1,),
                          lambda d: mg1T[0:17, tsl], lambda d: MGN, rf, ri, W,
                          ktok, 'ktok', qT, kT, tsl)


def stage_scan(C, f):
    S = C.S
    I = C.I
    Z = C.Z
    nkt = 1 if f == 0 else 2
    dv = 256 if f == 0 else 257
    hk = 4 * nkt * 128
    with ExitStack() as es2:
        def sbt(name, shape, dt):
            return es2.enter_context(C.nc.sbuf_tensor("s%d_%s" % (f, name), shape, dt))
        St = sbt("St", [128, 2, 4, nkt, dv], F32)
        Sb = sbt("Sb", [128, 2, 4, nkt, dv], BF16)
        OF = sbt("OF", [128, 16, 4, 256], F32)
        qtb = [[sbt("qt%d%d" % (d, i), [128, hk], BF16) for i in range(2)] for d in range(2)]
        ktb = [[sbt("kt%d%d" % (d, i), [128, hk], BF16) for i in range(2)] for d in range(2)]
        kdb = [[sbt("kd%d%d" % (d, i), [128, hk], BF16) for i in range(2)] for d in range(2)]
        vb = [[sbt("v%d%d" % (d, i), [128, 4 * dv], BF16) for i in range(2)] for d in range(2)]
        sTb = [sbt("sT%d" % i, [128, 128], BF16) for i in range(4)]
        gn = sbt("gn", [128, 1024], F32)
        yn = sbt("yn", [128, 4, 256], F32)
        tot4 = sbt("tot", [128, 4, 256], F32)
        junk4 = sbt("junk", [128, 4, 256], BF16)
        ss4 = sbt("ss", [128, 4], F32)
        rstd4 = sbt("rstd", [128, 4], F32)
        rr8 = sbt("rr", [128, 8], F32)
        S.dma('sp', gn[:], I['g_gla'] if f == 0 else I['g_m'], writes=['gn'])
        S.op('dve', lambda e: e.memset(St[:], 0.0),
             writes=[('St', d, h, k) for d in range(2) for h in range(4) for k in range(nkt)])
        S.op('dve', lambda e: e.memset(Sb[:], 0.0), writes=[('Sb', d, h) for d in range(2) for h in range(4)])
        cnt = [0, 0]
        sti = [0]

        cnt_l = [0, 0]

        def chunk_loads(d, c, full):
            i = cnt_l[d] % 2
            cnt_l[d] += 1
            tg = "%d%d" % (d, i)
            qt, kt_, kd, v = qtb[d][i], ktb[d][i], kdb[d][i], vb[d][i]
            if full:
                S.dma('sp', qt[:], Z['QT%d' % f][d, c], writes=['qt' + tg])
                S.dma('sp', kt_[:], Z['KT%d' % f][d, c], writes=['kt' + tg])
            S.dma('sp', kd[:], Z['KD%d' % f][d, c], writes=['kd' + tg])
            S.dma('sp', v[:], Z['V%d' % f][c], writes=['v' + tg])

        def chunk_step(d, c, full, last):
            i = cnt[d] % 2
            cnt[d] += 1
            tg = "%d%d" % (d, i)
            qt, kt_, kd, v = qtb[d][i], ktb[d][i], kdb[d][i], vb[d][i]
            hs = range(4)
            vhs = [v[:, h * dv:(h + 1) * dv] for h in hs]
            if full:
                os_, ons, ss_, sns = [], [], [], []
                for h in hs:
                    o, on = C.nb()
                    s, sn = C.nb()
                    for k in range(nkt):
                        qs = slice((h * nkt + k) * 128, (h * nkt + k + 1) * 128)
                        mm(C, o[:, 0:dv], qt[:, qs], Sb[:, d, h, k, :], k == 0, False, ['qt' + tg, ('Sb', d, h)], [on])
                    for k in range(nkt):
                        qs = slice((h * nkt + k) * 128, (h * nkt + k + 1) * 128)
                        mm(C, s[:, 0:128], kt_[:, qs], qt[:, qs], k == 0, k == nkt - 1, ['kt' + tg, 'qt' + tg], [sn])
                    os_.append(o)
                    ons.append(on)
                    ss_.append(s)
                    sns.append(sn)
                for h in hs:
                    tt(C, sTb[h][:], ss_[h][:, 0:128], C.maskb[:, d, :], ALU.mult, [sns[h], 'maskb'], ['sT%d' % h])
                for h in hs:
                    mm(C, os_[h][:, 0:dv], sTb[h][:], vhs[h], False, True, ['sT%d' % h, 'v' + tg], [ons[h]])
                for h in hs:
                    o, on = os_[h], ons[h]
                    tot = tot4[:, h, :]
                    junk = junk4[:, h, :]
                    ss = ss4[:, h:h + 1]
                    rstd = rstd4[:, h:h + 1]
                    rr = rr8[:, d * 4 + h:d * 4 + h + 1]
                    RRN = 'rr%d%d' % (d, h)
                    TOTN = 'tot%d' % h
                    if f == 1:
                        ts(C, rr, o[:, 256:257], -1.0, 1.0, ALU.mult, ALU.max, [on], [RRN])
                        tt(C, rr, rr, o[:, 256:257], ALU.max, [RRN, on], [RRN])
                        S.op('dve', lambda e: e.reciprocal(out=rr, in_=rr), reads=[RRN], writes=[RRN])
                    if d == 0:
                        if f == 0:
                            cp(C, 'act', OF[:, c, h, :], o[:, 0:256], [on], [('OF', c, h)])
                        else:
                            act(C, OF[:, c, h, :], o[:, 0:256], AF.Copy, [on, RRN], [('OF', c, h)], scale=rr)
                    else:
                        if f == 0:
                            tt(C, tot, OF[:, c, h, :], o[:, 0:256], ALU.add, [('OF', c, h), on], [TOTN])
                        else:
                            stt(C, tot, o[:, 0:256], rr, OF[:, c, h, :], ALU.mult, ALU.add,
                                [on, RRN, ('OF', c, h)], [TOTN])
                        rms_rstd(C, tot, TOTN, 256, junk, 'junk%d' % h, ss, rstd, 'sc%d_' % h)
                        stt(C, yn[:, h, :], tot, rstd, gn[:, h * 256:(h + 1) * 256], ALU.mult, ALU.mult,
                            [TOTN, 'sc%d_rstd' % h, 'gn'], [('yn', h)])
            if not last:
                us = []
                for h in hs:
                    for k in range(nkt):
                        u, un = C.nb()
                        ks = slice((h * nkt + k) * 128, (h * nkt + k + 1) * 128)
                        mm(C, u[:, 0:dv], kd[:, ks], vhs[h], True, True, ['kd' + tg, 'v' + tg], [un])
                        us.append((h, k, u, un))
                for (h, k, u, un) in us:
                    stt(C, St[:, d, h, k, :], St[:, d, h, k, :], C.elast[f][:, d, c, h:h + 1], u[:, 0:dv],
                        ALU.mult, ALU.add, [('St', d, h, k), ('elast', f), un], [('St', d, h, k)])
                    cp(C, 'act', Sb[:, d, h, k, :], St[:, d, h, k, :], [('St', d, h, k)], [('Sb', d, h)])
            if full and d == 1:
                S.dma('sp', Z['YN'][c, :, f * 1024:(f + 1) * 1024], yn[:].rearrange("p h e -> p (h e)"),
                      reads=[('yn', hh) for hh in range(4)], writes=[('YN', f, c)])

        steps = []
        for step in range(32):
            if step < 16:
                steps.append((0, step, True, step == 15))
            c1 = 31 - step
            steps.append((1, c1, c1 < 16, c1 == 0))
        per_dir = {0: [st for st in steps if st[0] == 0], 1: [st for st in steps if st[0] == 1]}
        done_l = [0, 0]
        done_c = [0, 0]
        for (d, c, full, last) in steps:
            while done_l[d] <= min(done_c[d] + 1, len(per_dir[d]) - 1):
                dd, cc, ff, _ = per_dir[d][done_l[d]]
                chunk_loads(dd, cc, ff)
                done_l[d] += 1
            chunk_step(d, c, full, last)
            done_c[d] += 1


def stage_post(C):
    S = C.S
    I = C.I
    Z = C.Z
    RT = C.RT
    with ExitStack() as es2:
        def sbt(name, shape, dt):
            return es2.enter_context(C.nc.sbuf_tensor("p_" + name, shape, dt))
        W = {}
        W['junk'] = sbt("junk", [128, D], BF16)
        W['ss'] = sbt("ss", [128, 1], F32)
        W['rstd'] = sbt("rstd", [128, 1], F32)
        gain = sbt("gain", [128, D], F32)
        xbuf = [sbt("x%d" % i, [128, D], F32) for i in range(2)]
        xn = sbt("xn", [128, D], F32)
        xnT = sbt("xnT", [128, KT, 512], BF16)
        big = sbt("big", [128, 4, D], F32)
        yT = sbt("yT", [128, KT, 512], BF16)
        mT = sbt("mT", [128, KT, 512], BF16)
        wall = sbt("wall", [128, 4, KT, 256], BF16)
        wbuf = [wall[:, i] for i in range(4)]
        wbig = [wall[:, 2 * j:2 * j + 2].rearrange("p a k c -> p (a k c)").rearrange("p (k c) -> p k c", c=512)
                for j in range(2)]
        kmT = sbt("kmT", [128, KT, 256], BF16)
        vm = sbt("vm", [128, 2, D], BF16)
        ynb = [sbt("ynb%d" % i, [128, 512], F32) for i in range(2)]
        gtmp = sbt("gtmp", [128, 512], F32)
        sga = sbt("sga", [128, 512], F32)
        sgb = sbt("sgb", [128, 512], F32)
        P4 = sbt("P", [128, 4, 256], F32)
        PT4 = sbt("PT", [128, 4, 2, 128], BF16)
        mxs4 = sbt("mxs", [128, 4], F32)
        rs4 = sbt("rs", [128, 4], F32)
        wrt = sbt("wrt", [128, KT, 36], F32)
        brt = sbt("brt", [128, 36], F32)
        jrow = sbt("jrow", [128, 4], F32)
        lg = sbt("lg", [128, 36], F32)
        sm = sbt("sm", [128, 16], F32)
        ge = sbt("ge", [128, 4], F32)
        ohg = sbt("ohg", [128, 4], F32)
        esel = sbt("esel", [128, 8], F32)
        top8 = sbt("top8", [128, 8], F32)
        idx8 = sbt("idx8", [128, 8], U32)
        if8 = sbt("if8", [128, 8], F32)

        S.dma('sp', wrt[:], I['w_rt'].rearrange("(k p) c -> p k c", p=128), writes=['wrt'])
        S.dma('sp', brt[:], I['b_rt'], writes=['brt'])
        S.dma('sp', jrow[:], I['c_jrow'], writes=['jrow'])

        wi = [0]

        def next_w(nm, blk, nk=KT):
            i = wi[0]
            wi[0] = (i + 1) % 4
            load_w_tiled(C, wbuf[i], 'w%d' % i, nm, blk, nk, 256)
            return wbuf[i], 'w%d' % i

        def next_w_direct(src_ap):
            i = wi[0]
            wi[0] = (i + 1) % 4
            load_w_block(C, wbuf[i], 'w%d' % i, src_ap, KT, 256)
            return wbuf[i], 'w%d' % i

        bi = [0]

        def next_wbig(nm, blk):
            j = bi[0]
            bi[0] = (j + 1) % 2
            names = ['w%d' % (2 * j), 'w%d' % (2 * j + 1)]
            C.S.dma('pool', wbig[j], C.Z['W_' + nm][blk], reads=[('Wc', nm, blk)], writes=names)
            return wbig[j], names

        yi = [0]

        def next_ynb():
            i = yi[0]
            yi[0] = (i + 1) % 2
            return ynb[i], 'ynb%d' % i

        S.dma('sp', gain[:], I['g_mem'], writes=['gain'])
        for mt in range(2):
            S.dma('sp', xbuf[mt][:], I['mem'][mt * 128:(mt + 1) * 128, :], writes=['x%d' % mt])
            norm_T(C, xbuf[mt][:], 'x%d' % mt, gain[:], 'gain', xn[:], 'xn', W, xnT, 'xnT',
                   slice(mt * 128, (mt + 1) * 128))
        for blk in range(8):
            w, wr = next_w_direct(I['w_xkv'][:, blk * 256:(blk + 1) * 256])
            for c2 in range(2):
                p, pn = C.nb()
                for k in range(KT):
                    mm(C, p[:, 0:256], w[:, k, c2 * 128:(c2 + 1) * 128], xnT[:, k, 0:256], k == 0, k == KT - 1,
                       [wr, 'xnT'], [pn])
                cp(C, 'act', kmT[:, blk * 2 + c2, :], p[:, 0:256], [pn], ['kmT'])
        for blk in range(8):
            w, wr = next_w_direct(I['w_xkv'][:, D + blk * 256:D + (blk + 1) * 256])
            for mt in range(2):
                p, pn = C.nb()
                for k in range(KT):
                    mm(C, p[:, 0:256], xnT[:, k, mt * 128:(mt + 1) * 128], w[:, k, :], k == 0, k == KT - 1,
                       ['xnT', wr], [pn])
                cp(C, 'dve', vm[:, mt, blk * 256:(blk + 1) * 256], p[:, 0:256], [pn], ['vm'])

        att_scale = float(512 ** -0.5)
        for g in range(4):
            tsls = [slice(j * 128, (j + 1) * 128) for j in range(4)]
            S.dma('sp', gain[:], I['g_mix'], writes=['gain'])
            for j in range(4):
                t = g * 4 + j
                xb, xr = xbuf[j % 2], 'x%d' % (j % 2)
                S.dma('sp', xb[:], I['x'][t * 128:(t + 1) * 128, :], writes=[xr])
                norm_T(C, xb[:], xr, gain[:], 'gain', xn[:], 'xn', W, xnT, 'xnT', tsls[j])
            for blk in range(4):
                w, wrs = next_wbig('gz', blk)
                cs_ = slice(blk * 512, (blk + 1) * 512)
                for j in range(4):
                    t = g * 4 + j
                    p, pn = C.nb()
                    for k in range(KT):
                        mm(C, p[:, :], xnT[:, k, tsls[j]], w[:, k, :], k == 0, k == KT - 1, ['xnT'] + wrs, [pn])
                    act(C, gtmp[:], p[:, :], AF.Silu if blk < 2 else AF.Sigmoid, [pn], ['gtmp'])
                    yb_, ybr = next_ynb()
                    S.dma('sp', yb_[:], Z['YN'][t, :, cs_], writes=[ybr])
                    tt(C, big[:, j, cs_], gtmp[:], yb_[:], ALU.mult, ['gtmp', ybr], [('big', j)])
            for j in range(4):
                transpose_to(C, big[:, j, :], ('big', j), KT, yT, 'yT', tsls[j])
            for cb in range(8):
                cs_ = slice(cb * 256, (cb + 1) * 256)
                wa, war = next_w('ba', cb, nk=8)
                wb_, wbr = next_w('bb', cb, nk=8)
                wga, wgar = next_w('gab', cb)
                wgb, wgbr = next_w('gab', 8 + cb)
                for c2 in range(2):
                    ct = cb * 2 + c2
                    c2s = slice(c2 * 128, (c2 + 1) * 128)
                    pa, pan = C.nb()
                    for k in range(8):
                        mm(C, pa[:, :], wa[:, k, c2s], yT[:, k, :], k == 0, k == 7, [war, 'yT'], [pan])
                    pb_, pbn = C.nb()
                    for k in range(8):
                        mm(C, pb_[:, :], wb_[:, k, c2s], yT[:, 8 + k, :], k == 0, k == 7, [wbr, 'yT'], [pbn])
                    ga, gan = C.nb()
                    for k in range(KT):
                        mm(C, ga[:, :], wga[:, k, c2s], xnT[:, k, :], k == 0, k == KT - 1, [wgar, 'xnT'], [gan])
                    gb, gbn = C.nb()
                    for k in range(KT):
                        mm(C, gb[:, :], wgb[:, k, c2s], xnT[:, k, :], k == 0, k == KT - 1, [wgbr, 'xnT'], [gbn])
                    act(C, sga[:], ga[:, :], AF.Sigmoid, [gan], ['sga'])
                    act(C, sgb[:], gb[:, :], AF.Sigmoid, [gbn], ['sgb'])
                    tt(C, sga[:], sga[:], pa[:, :], ALU.mult, ['sga', pan], ['sga'])
                    tt(C, sgb[:], sgb[:], pb_[:, :], ALU.mult, ['sgb', pbn], ['sgb'])
                    tt(C, mT[:, ct, :], sga[:], sgb[:], ALU.add, ['sga', 'sgb'], ['mT'])
            for cb in range(4):
                cs_ = slice(cb * 512, (cb + 1) * 512)
                w, wrs = next_wbig('mix', cb)
                for j in range(4):
                    t = g * 4 + j
                    p, pn = C.nb()
                    for k in range(KT):
                        mm(C, p[:, :], mT[:, k, tsls[j]], w[:, k, :], k == 0, k == KT - 1, ['mT'] + wrs, [pn])
                    yb_, ybr = next_ynb()
                    S.dma('sp', yb_[:], I['x'][t * 128:(t + 1) * 128, cs_], writes=[ybr])
                    tt(C, big[:, j, cs_], p[:, :], yb_[:], ALU.add, [pn, ybr], [('big', j)])
            S.dma('sp', gain[:], I['g_xattn'], writes=['gain'])
            for j in range(4):
                norm_T(C, big[:, j, :], ('big', j), gain[:], 'gain', xn[:], 'xn', W, xnT, 'xnT', tsls[j])
            for cb in range(8):
                w, wr = next_w('xq', cb)
                for c2 in range(2):
                    p, pn = C.nb()
                    for k in range(KT):
                        mm(C, p[:, :], w[:, k, c2 * 128:(c2 + 1) * 128], xnT[:, k, :], k == 0, k == KT - 1,
                           [wr, 'xnT'], [pn])
                    cp(C, 'act' if c2 == 0 else 'dve', yT[:, cb * 2 + c2, :], p[:, :], [pn], ['yT'])
            for j in range(4):
                ps_, pns = [], []
                for hd in range(4):
                    p, pn = C.nb()
                    ps_.append(p)
                    pns.append(pn)
                    for dt_ in range(4):
                        mm(C, p[:, 0:256], yT[:, hd * 4 + dt_, tsls[j]], kmT[:, hd * 4 + dt_, :], dt_ == 0, dt_ == 3,
                           ['yT', 'kmT'], [pn])
                for hd in range(4):
                    S.op('dve', lambda e: e.reduce_max(out=mxs4[:, hd:hd + 1], in_=ps_[hd][:, 0:256], axis=AX.X),
                         reads=[pns[hd]], writes=[('mxs', hd)])
                    ts(C, mxs4[:, hd:hd + 1], mxs4[:, hd:hd + 1], -att_scale, None, ALU.mult, None, [('mxs', hd)], [('mxs', hd)])
                for hd in range(4):
                    act(C, P4[:, hd, :], ps_[hd][:, 0:256], AF.Exp, [pns[hd], ('mxs', hd)], [('P', hd), ('rs', hd)],
                        scale=att_scale, bias=mxs4[:, hd:hd + 1], accum_out=rs4[:, hd:hd + 1])
                for hd in range(4):
                    pt_, ptn = C.nb()
                    for mt in range(2):
                        C.S.op('pe', lambda e: e.transpose(pt_[:, mt * 128:(mt + 1) * 128], P4[:, hd, mt * 128:(mt + 1) * 128],
                                                           C.ident[:]), reads=[('P', hd), 'ident'], writes=[ptn])
                    cp(C, 'dve' if hd % 2 == 0 else 'act', PT4[:, hd], pt_[:, 0:256].rearrange("p (m t) -> p m t", m=2),
                       [ptn], [('PT', hd)])
                for hd in range(4):
                    po, pon = C.nb()
                    for mt in range(2):
                        mm(C, po[:, :], PT4[:, hd, mt, :], vm[:, mt, hd * 512:(hd + 1) * 512], mt == 0, mt == 1,
                           [('PT', hd), 'vm'], [pon])
                    S.op('dve', lambda e: e.reciprocal(out=rs4[:, hd:hd + 1], in_=rs4[:, hd:hd + 1]),
                         reads=[('rs', hd)], writes=[('rs', hd)])
                    act(C, xn[:, hd * 512:(hd + 1) * 512], po[:, :], AF.Copy, [pon, ('rs', hd)], ['xn'], scale=rs4[:, hd:hd + 1])
                transpose_to(C, xn[:], 'xn', KT, mT, 'mT', tsls[j])
            for cb in range(4):
                cs_ = slice(cb * 512, (cb + 1) * 512)
                w, wrs = next_wbig('xo', cb)
                for j in range(4):
                    p, pn = C.nb()
                    for k in range(KT):
                        mm(C, p[:, :], mT[:, k, tsls[j]], w[:, k, :], k == 0, k == KT - 1, ['mT'] + wrs, [pn])
                    tt(C, big[:, j, cs_], big[:, j, cs_], p[:, :], ALU.add, [('big', j), pn], [('big', j)])
            S.dma('sp', gain[:], I['g_ffn'], writes=['gain'])
            x3T = xbuf[1][:].rearrange("p (k t) -> p k t", k=KT)
            for j in range(4):
                t = g * 4 + j
                S.dma('sp', Z['H2'][t], big[:, j, :], reads=[('big', j)], writes=[('H2', t)])
                norm_T(C, big[:, j, :], ('big', j), gain[:], 'gain', xn[:], 'xn', W, x3T, 'x1', slice(0, 128))
                S.dma('sp', Z['XN3'][t], xn[:], reads=['xn'], writes=[('XN3', t)])
                p, pn = C.nb()
                for k in range(KT):
                    mm(C, p[:, 0:36], x3T[:, k, :], wrt[:, k, :], k == 0, k == KT - 1, ['x1', 'wrt'], [pn])
                tt(C, lg[:], p[:, 0:36], brt[:], ALU.add, [pn, 'brt'], ['lg'])
                S.op('dve', lambda e: e.reduce_max(out=sm[:, 0:1], in_=lg[:, 0:4], axis=AX.X), reads=['lg'], writes=['sm'])
                ts(C, ohg[:], lg[:, 0:4], sm[:, 0:1], None, ALU.is_equal, None, ['lg', 'sm'], ['ohg'])
                ts(C, sm[:, 1:2], sm[:, 0:1], -1.0, None, ALU.mult, None, ['sm'], ['sm'])
                act(C, ge[:], lg[:, 0:4], AF.Exp, ['lg', 'sm'], ['ge', 'sm'], bias=sm[:, 1:2], scale=1.0,
                    accum_out=sm[:, 2:3])
                S.op('dve', lambda e: e.reciprocal(out=sm[:, 3:4], in_=sm[:, 2:3]), reads=['sm'], writes=['sm'])
                tt(C, ge[:], ohg[:], jrow[:], ALU.mult, ['ohg', 'jrow'], ['ge'])
                S.op('dve', lambda e: e.reduce_sum(out=sm[:, 4:5], in_=ge[:], axis=AX.X), reads=['ge'], writes=['sm'])
                ts(C, esel[:], lg[:, 4:12], ohg[:, 0:1], None, ALU.mult, None, ['lg', 'ohg'], ['esel'])
                for gj in range(1, 4):
                    stt(C, esel[:], lg[:, 4 + 8 * gj:12 + 8 * gj], ohg[:, gj:gj + 1], esel[:], ALU.mult, ALU.add,
                        ['lg', 'ohg', 'esel'], ['esel'])
                S.op('dve', lambda e: e.max(out=top8[:], in_=esel[:]), reads=['esel'], writes=['top8'])
                S.op('dve', lambda e: e.max_index(out=idx8[:], in_max=top8[:], in_values=esel[:]),
                     reads=['top8', 'esel'], writes=['idx8'])
                cp(C, 'dve', if8[:], idx8[:], ['idx8'], ['if8'])
                tt(C, sm[:, 5:6], top8[:, 1:2], top8[:, 0:1], ALU.subtract, ['top8'], ['sm'])
                act(C, sm[:, 6:7], sm[:, 5:6], AF.Exp, ['sm'], ['sm'])
                ts(C, sm[:, 7:8], sm[:, 6:7], 1.0, None, ALU.add, None, ['sm'], ['sm'])
                S.op('dve', lambda e: e.reciprocal(out=sm[:, 7:8], in_=sm[:, 7:8]), reads=['sm'], writes=['sm'])
                tt(C, RT[:, t, 2:3], sm[:, 3:4], sm[:, 7:8], ALU.mult, ['sm'], ['RT'])
                tt(C, RT[:, t, 3:4], RT[:, t, 2:3], sm[:, 6:7], ALU.mult, ['RT', 'sm'], ['RT'])
                stt(C, RT[:, t, 0:1], sm[:, 4:5], 8.0, if8[:, 0:1], ALU.mult, ALU.add, ['sm', 'if8'], ['RT'])
                stt(C, RT[:, t, 1:2], sm[:, 4:5], 8.0, if8[:, 1:2], ALU.mult, ALU.add, ['sm', 'if8'], ['RT'])


def stage_moe(C):
    S = C.S
    I = C.I
    Z = C.Z
    RT = C.RT
    nc = C.nc
    with ExitStack() as es1:
        def sbp(name, shape, dt):
            return es1.enter_context(C.nc.sbuf_tensor("m_" + name, shape, dt))
        idx = sbp("idx", [128, NT_OWN, 2], I32)
        bei = sbp("bei", [1, 64], I32)
        with ExitStack() as es2:
            def sbt(name, shape, dt):
                return es2.enter_context(C.nc.sbuf_tensor("md_" + name, shape, dt))
            iota = sbt("iota", [128, 32], F32)
            brow = sbt("brow", [32, 64], F32)
            M = sbt("M", [128, NT_OWN, 32], F32)
            cf = sbt("cf", [128, 32], F32)
            ci = sbt("ci", [128, 32], I32)
            padf = sbt("padf", [128, 32], F32)
            pend = sbt("pend", [128, 32], F32)
            pst = sbt("pst", [128, 32], F32)
            dst = sbt("dst", [128, 32], F32)
            oh = sbt("oh", [128, 32], F32)
            cT = sbt("cT", [32, 1], F32)
            cTi = sbt("cTi", [32, 1], I32)
            pT = sbt("pT", [32, 1], F32)
            cmp_ = sbt("cmp", [32, 64], F32)
            bef = sbt("bef", [1, 64], F32)
            neq = sbt("neq", [1, 64], F32)
            ldf = sbt("ldf", [1, 64], F32)
            xr = [sbt("xr%d" % i, [128, D], F32) for i in range(2)]
            S.dma('sp', iota[:], I['c_iota32'], writes=['iota'])
            S.dma('sp', brow[:], I['c_brow'], writes=['brow'])
            for t in range(NT_OWN):
                ts(C, M[:, t, :], iota[:], RT[:, t, 0:1], None, ALU.is_equal, None, ['iota', 'RT'], [('M', t)])
                stt(C, M[:, t, :], iota[:], RT[:, t, 1:2], M[:, t, :], ALU.is_equal, ALU.add, ['iota', 'RT', ('M', t)], [('M', t)])
            p, pn = C.nb()
            for t in range(NT_OWN):
                mm(C, p[:, 0:32], C.ones[:], M[:, t, :], t == 0, t == NT_OWN - 1, ['ones', ('M', t)], [pn])
            ts(C, cf[:], p[:, 0:32], 127.0, None, ALU.add, None, [pn], ['cf'])
            cp(C, 'dve', ci[:], cf[:], ['cf'], ['ci'])
            ts(C, ci[:], ci[:], 7, None, ALU.arith_shift_right, None, ['ci'], ['ci'])
            ts(C, ci[:], ci[:], 7, None, ALU.logical_shift_left, None, ['ci'], ['ci'])
            cp(C, 'dve', padf[:], ci[:], ['ci'], ['padf'])
            S.op('dve', lambda e: e.tensor_tensor_scan(out=pend[:], data0=C.ones[:, 0:32], data1=padf[:], initial=0.0,
                                                      op0=ALU.mult, op1=ALU.add), reads=['ones', 'padf'], writes=['pend'])
            tt(C, pst[:], pend[:], padf[:], ALU.subtract, ['pend', 'padf'], ['pst'])
            for t in range(NT_OWN):
                p, pn = C.nb()
                for j in range(t):
                    mm(C, p[:, 0:32], C.ones[:], M[:, j, :], j == 0, False, ['ones', ('M', j)], [pn])
                mm(C, p[:, 0:32], C.ustr[:, 1, :], M[:, t, :], t == 0, True, ['ustr', ('M', t)], [pn])
                tt(C, dst[:], p[:, 0:32], pst[:], ALU.add, [pn, 'pst'], ['dst'])
                for a_i in range(2):
                    ts(C, oh[:], iota[:], RT[:, t, a_i:a_i + 1], None, ALU.is_equal, None, ['iota', 'RT'], ['oh'])
                    tt(C, oh[:], oh[:], dst[:], ALU.mult, ['oh', 'dst'], ['oh'])
                    S.op('dve', lambda e: e.reduce_sum(out=RT[:, t, 4 + a_i:5 + a_i], in_=oh[:], axis=AX.X),
                         reads=['oh'], writes=['RT'])
            cp(C, 'dve', idx[:], RT[:, :, 4:6], ['RT'], ['idx'])
            for t in range(NT_OWN):
                S.dma('sp', Z['RT'][t], RT[:, t, :], reads=['RT'], writes=[('RTd', t)])
            p, pn = C.nb()
            for t in range(NT_OWN):
                mm(C, p[0:32, 0:1], M[:, t, :], C.ones[:, 0:1], t == 0, t == NT_OWN - 1, [('M', t), 'ones'], [pn])
            ts(C, cT[:], p[0:32, 0:1], 127.0, None, ALU.add, None, [pn], ['cT'])
            cp(C, 'dve', cTi[:], cT[:], ['cT'], ['cTi'])
            ts(C, cTi[:], cTi[:], 7, None, ALU.arith_shift_right, None, ['cTi'], ['cTi'])
            ts(C, cTi[:], cTi[:], 7, None, ALU.logical_shift_left, None, ['cTi'], ['cTi'])
            cp(C, 'dve', cT[:], cTi[:], ['cTi'], ['cT'])
            p, pn = C.nb()
            mm(C, p[0:32, 0:1], C.mincl[0:32, 0, 0:32], cT[:], True, True, ['mincl', 'cT'], [pn])
            cp(C, 'dve', pT[:], p[0:32, 0:1], [pn], ['pT'])
            ts(C, cmp_[:], brow[:], pT[:, 0:1], None, ALU.is_ge, None, ['brow', 'pT'], ['cmp'])
            p, pn = C.nb()
            mm(C, p[0:1, 0:64], C.ones[0:32, 0:1], cmp_[:], True, True, ['ones', 'cmp'], [pn])
            cp(C, 'dve', bef[:], p[0:1, 0:64], [pn], ['bef'])
            S.op('dve', lambda e: e.memset(neq[:], 1.0), writes=['neq'])
            tt(C, neq[0:1, 1:64], bef[0:1, 1:64], bef[0:1, 0:63], ALU.not_equal, ['bef'], ['neq'])
            ts(C, ldf[:], bef[:], -40.0, None, ALU.add, None, ['bef'], ['ldf'])
            tt(C, ldf[:], ldf[:], neq[:], ALU.mult, ['ldf', 'neq'], ['ldf'])
            ts(C, ldf[:], ldf[:], 40.0, None, ALU.add, None, ['ldf'], ['ldf'])
            cp(C, 'dve', bei[:], ldf[:], ['ldf'], ['bei'])
            S.dma('sp', Z['BE'], bef[:], reads=['bef'], writes=['BEd'])
            for t in range(NT_OWN):
                xb, xbr = xr[t % 2], 'xr%d' % (t % 2)
                S.dma('sp', xb[:], Z['XN3'][t], writes=[xbr])
                for a_i in range(2):
                    S.dma_indirect('pool', out=Z['XR'], out_offset=bass.IndirectOffsetOnAxis(ap=idx[:, t, a_i:a_i + 1], axis=0),
                                   in_=xb[:], in_offset=None, reads=[xbr, 'idx'] + [('XRz', b) for b in range(NBLK)],
                                   writes=[('XR', b) for b in range(NBLK)])
            S.barrier()
        with ExitStack() as es2:
            def sbt(name, shape, dt):
                return es2.enter_context(C.nc.sbuf_tensor("me_" + name, shape, dt))
            wgs = sbt("wgs", [128, KT, 512], F32)
            wus = sbt("wus", [128, KT, 512], F32)
            wds = sbt("wds", [128, 4, D], F32)
            wg = sbt("wg", [128, KT, 512], BF16)
            wu = sbt("wu", [128, KT, 512], BF16)
            wd = sbt("wd", [128, 4, D], BF16)
            x2 = [sbt("x%d" % i, [128, D], F32) for i in range(2)]
            y2 = [sbt("y%d" % i, [128, D], F32) for i in range(2)]
            xrT = sbt("xrT", [128, KT, 128], BF16)
            a_ = sbt("a", [128, 512], F32)
            aT = sbt("aT", [128, 4, 128], BF16)

            def issue_loads(b):
                q = ('pool', 'sp', 'act')[b % 3] if b < 60 else ('sp', 'act')[b % 2]
                S.wait_for(q, ['bei'])
                ev = S.eng[q].value_load(bei[0:1, b:b + 1])
                for (wt, wn, src) in ((wgs, 'wgs', 'w_gate'), (wus, 'wus', 'w_up'), (wds, 'wds', 'w_down')):
                    S.dma(q, wt[:], I[src].rearrange("e (p k) f -> e p k f", p=128)[bass.ds(ev, 1)].rearrange("a p k f -> (a p) k f"),
                          reads=['bei'], writes=[wn], bounds_check="skip_entire_dma")
                S.eng[q].free_register(ev.val)

            def issue_x(b):
                S.dma('sp', x2[b % 2][:], Z['XR'][b * 128:(b + 1) * 128, :], reads=[('XR', b)], writes=['x%d' % (b % 2)])

            issue_loads(0)
            issue_x(0)
            for b in range(NBLK):
                xb, xbr = x2[b % 2], 'x%d' % (b % 2)
                yb, ybr = y2[b % 2], 'y%d' % (b % 2)
                cp(C, 'dve', wg[:, 0:8, :], wgs[:, 0:8, :], ['wgs'], ['wg_lo'])
                cp(C, 'act', wg[:, 8:16, :], wgs[:, 8:16, :], ['wgs'], ['wg_hi'])
                cp(C, 'dve', wu[:, 0:8, :], wus[:, 0:8, :], ['wus'], ['wu_lo'])
                cp(C, 'act', wu[:, 8:16, :], wus[:, 8:16, :], ['wus'], ['wu_hi'])
                cp(C, 'dve', wd[:, 0:2, :], wds[:, 0:2, :], ['wds'], ['wd0'])
                cp(C, 'act', wd[:, 2:4, :], wds[:, 2:4, :], ['wds'], ['wd1'])
                if b + 1 < NBLK:
                    issue_loads(b + 1)
                    issue_x(b + 1)
                transpose_to(C, xb[:], xbr, KT, xrT, 'xrT', slice(0, 128), interleave=True)
                hg, hgn = C.nb()
                for k in range(KT):
                    mm(C, hg[:, :], xrT[:, k, :], wg[:, k, :], k == 0, k == KT - 1, ['xrT', 'wg_lo' if k < 8 else 'wg_hi'], [hgn])
                hu, hun = C.nb()
                for k in range(KT):
                    mm(C, hu[:, :], xrT[:, k, :], wu[:, k, :], k == 0, k == KT - 1, ['xrT', 'wu_lo' if k < 8 else 'wu_hi'], [hun])
                act(C, a_[:], hg[:, :], AF.Silu, [hgn], ['a'])
                tt(C, a_[:], a_[:], hu[:, :], ALU.mult, ['a', hun], ['a'])
                transpose_to(C, a_[:], 'a', 4, aT, 'aT', slice(0, 128), interleave=True)
                for cb in range(4):
                    y, yn_ = C.nb()
                    for k in range(4):
                        mm(C, y[:, :], aT[:, k, :], wd[:, k, cb * 512:(cb + 1) * 512], k == 0, k == 3,
                           ['aT', 'wd0' if k < 2 else 'wd1'], [yn_])
                    cp(C, 'act' if cb % 2 == 0 else 'dve', yb[:, cb * 512:(cb + 1) * 512], y[:, :], [yn_], [ybr])
                S.dma('sp', Z['YR'][b * 128:(b + 1) * 128, :], yb[:], reads=[ybr], writes=[('YR', b)])
            S.barrier()
        with ExitStack() as es2:
            def sbt(name, shape, dt):
                return es2.enter_context(C.nc.sbuf_tensor("mc_" + name, shape, dt))
            W = {}
            W['junk'] = sbt("junk", [128, D], BF16)
            W['ss'] = sbt("ss", [128, 1], F32)
            W['rstd'] = sbt("rstd", [128, 1], F32)
            gain = sbt("gain", [128, D], F32)
            NBUF = 3
            rb = [[sbt("r%d%d" % (a_i, i), [128, D], F32) for i in range(NBUF)] for a_i in range(2)]
            hb = [sbt("h%d" % i, [128, D], F32) for i in range(NBUF)]
            S.dma('sp', gain[:], I['g_final'], writes=['gain'])
            for t in range(NT_OWN):
                i = t % NBUF
                r0, r0n = rb[0][i], 'r0%d' % i
                r1, r1n = rb[1][i], 'r1%d' % i
                h2, h2n = hb[i], 'h%d' % i
                S.dma_indirect('pool', out=r0[:], out_offset=None, in_=Z['YR'],
                               in_offset=bass.IndirectOffsetOnAxis(ap=idx[:, t, 0:1], axis=0),
                               reads=['idx'], writes=[r0n])
                S.dma_indirect('pool', out=r1[:], out_offset=None, in_=Z['YR'],
                               in_offset=bass.IndirectOffsetOnAxis(ap=idx[:, t, 1:2], axis=0),
                               reads=['idx'], writes=[r1n])
                S.dma('pool', h2[:], Z['H2'][t], writes=[h2n])
                stt(C, h2[:], r0[:], RT[:, t, 2:3], h2[:], ALU.mult, ALU.add, [r0n, 'RT', h2n], [h2n])
                stt(C, h2[:], r1[:], RT[:, t, 3:4], h2[:], ALU.mult, ALU.add, [r1n, 'RT', h2n], [h2n])
                rms_rstd(C, h2[:], h2n, D, W['junk'][:], 'junk', W['ss'][:], W['rstd'][:], 'fn_')
                stt(C, h2[:], h2[:], W['rstd'][:, 0:1], gain[:], ALU.mult, ALU.mult, [h2n, 'fn_rstd', 'gain'], [h2n])
                S.dma('sp', C.out[t * 128:(t + 1) * 128, :], h2[:], reads=[h2n], writes=['out'])
```

```python
import numpy as np
from contextlib import ExitStack
import concourse.bass as bass
import concourse.mybir as mybir
from concourse.bass_utils import run_bass_kernel_spmd

F32 = mybir.dt.float32
BF16 = mybir.dt.bfloat16
I32 = mybir.dt.int32
U32 = mybir.dt.uint32
F32R = mybir.dt.float32r
AF = mybir.ActivationFunctionType
ALU = mybir.AluOpType
AX = mybir.AxisListType


class Sched:
    def __init__(self, nc, es, n_dma=6):
        self.nc = nc
        self.es = es
        self.eng = {'pe': nc.tensor, 'dve': nc.vector, 'act': nc.scalar,
                    'pool': nc.gpsimd, 'sp': nc.sync}
        self.sem = {}
        self.cnt = {}
        for k in ('pe', 'dve', 'act', 'pool'):
            self.sem[k] = es.enter_context(nc.semaphore("s_" + k))
            self.cnt[k] = 0
        self.known = {k: {} for k in self.eng}
        self.dsem = {}
        self.dpos = {}
        for q in ('sp', 'pool', 'act'):
            n = {'sp': 16, 'pool': 12, 'act': 8}[q]
            self.dsem[q] = [[es.enter_context(nc.semaphore("d_%s%d" % (q, i))), 0] for i in range(n)]
            self.dpos[q] = 0
        self.dsem['bg'] = [[es.enter_context(nc.semaphore("d_bg%d" % i)), 0] for i in range(2)]
        self.dpos['bg'] = 0
        self.res_w = {}
        self.res_r = {}
        self.persist = {}
        self.semname = {}
        self.n_inst = 0

    def sb(self, name, shape, dt):
        return self.es.enter_context(self.nc.sbuf_tensor(name, shape, dt))

    def ps(self, name, shape, dt):
        return self.es.enter_context(self.nc.psum_tensor(name, shape, dt))

    def _deps(self, reads, writes):
        evs = []
        for r in reads:
            w = self.res_w.get(r)
            if w is not None:
                evs.append(w)
            w = self.persist.get(r)
            if w is not None:
                evs.append(w)
        for w_ in writes:
            w = self.res_w.get(w_)
            if w is not None:
                evs.append(w)
            evs.extend(self.res_r.get(w_, {}).values())
        return evs

    def _wait(self, e, evs, skip_own=False):
        kn = self.known[e]
        best = {}
        for (sem, val, key) in evs:
            if skip_own and key == e:
                continue
            if kn.get(key, 0) >= val:
                continue
            if key not in best or best[key][1] < val:
                best[key] = (sem, val)
        for key, (sem, val) in best.items():
            self.eng[e].wait_ge(sem, val)
            kn[key] = val
            self.n_inst += 1

    def _record(self, ev, reads, writes):
        for r in reads:
            d = self.res_r.setdefault(r, {})
            old = d.get(ev[2])
            if old is None or old[1] < ev[1]:
                d[ev[2]] = ev
        for w_ in writes:
            self.res_w[w_] = ev
            self.res_r[w_] = {}

    def wait_for(self, e, reads=(), writes=()):
        self._wait(e, self._deps(reads, writes))

    def op(self, e, fn, reads=(), writes=()):
        self._wait(e, self._deps(reads, writes), skip_own=(e == 'pe'))
        inst = fn(self.eng[e])
        self.cnt[e] += 1
        inst.then_inc(self.sem[e], 1)
        ev = (self.sem[e], self.cnt[e], e)
        self._record(ev, reads, writes)
        self.n_inst += 1
        return inst

    def _dma_slot(self, q):
        slots = self.dsem[q]
        i = self.dpos[q]
        self.dpos[q] = (i + 1) % len(slots)
        return i, slots[i]

    def dma(self, q, out, in_, reads=(), writes=(), **kw):
        i, slot = self._dma_slot(q)
        key = "d_%s%d" % (q, i)
        evs = self._deps(reads, writes)
        if slot[1] > 0:
            evs.append((slot[0], slot[1], key))
        self._wait(q, evs)
        inst = self.eng[q].dma_start(out=out, in_=in_, **kw)
        slot[1] += 16
        inst.then_inc(slot[0], 16)
        ev = (slot[0], slot[1], key)
        self._record(ev, reads, writes)
        self.n_inst += 1
        return inst

    def dma_bg(self, out, in_, reads=(), writes=()):
        i, slot = self._dma_slot('bg')
        key = "d_bg%d" % i
        evs = self._deps(reads, [])
        if slot[1] > 0:
            evs.append((slot[0], slot[1], key))
        self._wait('pool', evs)
        inst = self.eng['pool'].dma_start(out=out, in_=in_)
        slot[1] += 16
        inst.then_inc(slot[0], 16)
        ev = (slot[0], slot[1], key)
        for w_ in writes:
            self.persist[w_] = ev
        self.n_inst += 1
        return inst

    def dma_indirect(self, q, out, out_offset, in_, in_offset, reads=(), writes=(), **kw):
        assert q == 'pool'
        i, slot = self._dma_slot(q)
        key = "d_%s%d" % (q, i)
        evs = self._deps(reads, writes)
        if slot[1] > 0:
            evs.append((slot[0], slot[1], key))
        self._wait(q, evs)
        inst = self.eng[q].indirect_dma_start(out=out, out_offset=out_offset, in_=in_,
                                              in_offset=in_offset, **kw)
        slot[1] += 16
        inst.then_inc(slot[0], 16)
        ev = (slot[0], slot[1], key)
        self._record(ev, reads, writes)
        self.n_inst += 1
        return inst

    def barrier(self):
        evs = []
        for k in ('pe', 'dve', 'act', 'pool'):
            if self.cnt[k] > 0:
                evs.append((self.sem[k], self.cnt[k], k))
        for q in self.dsem:
            if q == 'bg':
                continue
            for i, slot in enumerate(self.dsem[q]):
                if slot[1] > 0:
                    evs.append((slot[0], slot[1], "d_%s%d" % (q, i)))
        for e in ('pe', 'dve', 'act', 'pool', 'sp'):
            self._wait(e, evs)
        self.res_w = {}
        self.res_r = {}

    def finish(self, outs):
        self._wait('sp', self._deps(outs, []))
        evs = []
        for k in ('pe', 'dve', 'act', 'pool'):
            if self.cnt[k] > 0:
                evs.append((self.sem[k], self.cnt[k], k))
        for q in self.dsem:
            for i, slot in enumerate(self.dsem[q]):
                if slot[1] > 0:
                    evs.append((slot[0], slot[1], "d_%s%d" % (q, i)))
        self._wait('sp', evs)


D = 2048
SEQ = 4096
NB = 4
OWN = 2048
NT_OWN = 16
NT_ALL = 32
EPS = 1e-6
KT = 16
NEXP = 32
NBLK = 64
NROWS = NBLK * 128
import math
LN_QS = math.log(128 ** -0.5)
LN_KS = math.log(256 ** -0.5)

DEBUG = False
WCONV_POST = (('gz', 4, 16, 512), ('ba', 8, 8, 256), ('bb', 8, 8, 256), ('gab', 16, 16, 256),
              ('mix', 4, 16, 512), ('xq', 8, 16, 256), ('xo', 4, 16, 512))
WCONV_A = (('q', 1, 16, 512), ('k', 1, 16, 512), ('v', 2, 16, 512), ('mx', 2, 16, 512))
WSRC = {'xkv': 'w_xkv', 'gz': 'w_gz', 'ba': 'w_ba', 'bb': 'w_bb', 'gab': 'w_gab', 'mix': 'w_mix',
        'xq': 'w_xq', 'xo': 'w_xo', 'q': 'w_q', 'k': 'w_k', 'v': 'w_v', 'mx': 'w_mx'}
STOP_AFTER = 99


class Ctx:
    pass


def build_program(debug=False, stop_after=99):
    nc = bass.Bass("TRN2", target_bir_lowering=False)
    C = Ctx()
    C.nc = nc

    def din(name, shape, dt=F32):
        return nc.dram_tensor(name, list(shape), dt, kind="ExternalInput").ap()

    def dscr(name, shape, dt=F32, dbg=False):
        kind = "ExternalOutput" if (debug and dbg) else "Internal"
        return nc.dram_tensor(name, list(shape), dt, kind=kind).ap()

    I = {}
    I['x'] = din('x', [SEQ, D])
    I['mem'] = din('mem', [256, D])
    for nm in ('g_mix', 'g_xattn', 'g_mem', 'g_ffn', 'g_final'):
        I[nm] = din(nm, [128, D])
    I['g_gla'] = din('g_gla', [128, 1024])
    I['g_m'] = din('g_m', [128, 1024])
    I['w_q'] = din('w_q', [D, 512])
    I['w_k'] = din('w_k', [D, 512])
    I['w_v'] = din('w_v', [D, 1024])
    I['w_mx'] = din('w_mx', [D, 1024])
    I['w_sm'] = din('w_sm', [D, 48])
    I['w_gz'] = din('w_gz', [D, 2048])
    I['w_gab'] = din('w_gab', [D, 4096])
    I['wlr1'] = din('wlr1', [2, 17, 512])
    I['rf1'] = din('rf1', [2, 17, 512])
    I['ri1'] = din('ri1', [2, 17, 512])
    I['conv_w'] = din('conv_w', [128, 8, 5])
    I['conv_b'] = din('conv_b', [128, 8])
    I['m_wq'] = din('m_wq', [4, 256, 256])
    I['m_wk'] = din('m_wk', [4, 256, 256])
    I['m_wv'] = din('m_wv', [4, 256, 256])
    I['w_ba'] = din('w_ba', [1024, D])
    I['w_bb'] = din('w_bb', [1024, D])
    I['w_mix'] = din('w_mix', [D, D])
    I['w_xq'] = din('w_xq', [D, D])
    I['w_xkv'] = din('w_xkv', [D, 2 * D])
    I['w_xo'] = din('w_xo', [D, D])
    I['w_rt'] = din('w_rt', [D, 36])
    I['b_rt'] = din('b_rt', [128, 36])
    I['w_gate'] = din('w_gate', [NEXP, D, 512])
    I['w_up'] = din('w_up', [NEXP, D, 512])
    I['w_down'] = din('w_down', [NEXP, 512, D])
    I['c_ident'] = din('c_ident', [128, 128])
    I['c_negi'] = din('c_negi', [128, 128])
    I['c_ones'] = din('c_ones', [128, 128])
    I['c_mincl'] = din('c_mincl', [2, 128, 128])
    I['c_ustr'] = din('c_ustr', [2, 128, 128])
    I['c_iota32'] = din('c_iota32', [128, 32])
    I['c_brow'] = din('c_brow', [32, 64])
    I['c_jrow'] = din('c_jrow', [128, 4])
    I['c_zero'] = din('c_zero', [128, D])
    out = nc.dram_tensor('out', [OWN, D], F32, kind="ExternalOutput").ap()

    Z = {}
    Z['mxT'] = dscr('z_mxT', [8, 128, SEQ + 4], F32, dbg=True)
    Z['mgT'] = dscr('z_mgT', [16, SEQ], F32, dbg=True)
    for f, (hk, dv) in enumerate(((4 * 128, 1024), (4 * 256, 4 * 257))):
        Z['QT%d' % f] = dscr('z_QT%d' % f, [2, NT_OWN, 128, hk], BF16, dbg=True)
        Z['KT%d' % f] = dscr('z_KT%d' % f, [2, NT_OWN, 128, hk], BF16, dbg=True)
        Z['KD%d' % f] = dscr('z_KD%d' % f, [2, NT_ALL, 128, hk], BF16, dbg=True)
        Z['V%d' % f] = dscr('z_V%d' % f, [NT_ALL, 128, dv], BF16, dbg=True)
    Z['YN'] = dscr('z_YN', [NT_OWN, 128, 2048], F32, dbg=True)
    for nm, nblk, nk, ncol in WCONV_POST + WCONV_A:
        Z['W_' + nm] = dscr('z_W_' + nm, [nblk, 128, nk, ncol], BF16)
    Z['H2'] = dscr('z_H2', [NT_OWN, 128, D], F32, dbg=True)
    Z['XN3'] = dscr('z_XN3', [NT_OWN, 128, D], F32, dbg=True)
    Z['XR'] = dscr('z_XR', [NROWS, D], F32)
    Z['YR'] = dscr('z_YR', [NROWS, D], F32)
    Z['RT'] = dscr('z_RT', [NT_OWN, 128, 8], F32, dbg=True)
    Z['BE'] = dscr('z_BE', [1, 64], F32, dbg=True)

    with ExitStack() as es:
        S = Sched(nc, es)
        C.S = S
        pb = [S.ps("pb%d" % i, [128, 512], F32) for i in range(8)]
        C.pbi = 0

        def nb():
            i = C.pbi
            C.pbi = (i + 1) % 8
            return pb[i], "pb%d" % i
        C.nb = nb

        ident = S.sb("ident", [128, 128], F32)
        negi = S.sb("negi", [128, 128], F32)
        ones = S.sb("ones", [128, 128], F32)
        mincl = S.sb("mincl", [128, 2, 128], F32)
        ustr = S.sb("ustr", [128, 2, 128], F32)
        maskb = S.sb("maskb", [128, 2, 128], F32)
        S.dma('sp', ident[:], I['c_ident'], writes=['ident'])
        S.dma('sp', negi[:], I['c_negi'], writes=['negi'])
        S.dma('sp', ones[:], I['c_ones'], writes=['ones'])
        for d in range(2):
            S.dma('sp', mincl[:, d, :], I['c_mincl'][d], writes=['mincl'])
            S.dma('sp', ustr[:, d, :], I['c_ustr'][d], writes=['ustr'])
            S.dma('sp', maskb[:, d, :], I['c_mincl'][d], writes=['maskb'])
        elast = [S.sb("elast%d" % f, [128, 2, NT_ALL, 4], F32) for f in range(2)]
        C.RT = S.sb("RT", [128, NT_OWN, 8], F32)
        S.op('dve', lambda e: e.memset(C.RT[:], 0.0), writes=['RT'])
        C.__dict__.update(dict(ident=ident, negi=negi, ones=ones, mincl=mincl, ustr=ustr,
                               maskb=maskb, elast=elast, pb=pb, I=I, Z=Z, out=out))

        stage_A(C, stop_after)
        if stop_after >= 2:
            S.barrier()
            stage_B(C)
        if stop_after >= 3:
            S.barrier()
            stage_scan(C, 0)
            S.barrier()
            stage_scan(C, 1)
        if stop_after >= 4:
            S.barrier()
            stage_post(C)
        if stop_after >= 5:
            S.barrier()
            stage_moe(C)
        outs = ['out'] if stop_after >= 5 else []
        S.barrier()
        S.finish(outs)
    return nc


def mm(C, out, lhsT, rhs, start, stop, reads, writes):
    C.S.op('pe', lambda e: e.matmul(out, lhsT=lhsT, rhs=rhs, start=start, stop=stop),
           reads=reads, writes=writes)


def act(C, out, in_, func, reads, writes, **kw):
    C.S.op('act', lambda e: e.activation(out=out, in_=in_, func=func, **kw), reads=reads, writes=writes)


def tt(C, out, in0, in1, op, reads, writes, eng='dve'):
    C.S.op(eng, lambda e: e.tensor_tensor(out=out, in0=in0, in1=in1, op=op), reads=reads, writes=writes)


def stt(C, out, in0, scalar, in1, op0, op1, reads, writes):
    C.S.op('dve', lambda e: e.scalar_tensor_tensor(out=out, in0=in0, scalar=scalar, in1=in1, op0=op0, op1=op1),
           reads=reads, writes=writes)


def ts(C, out, in0, s1, s2, op0, op1, reads, writes):
    if op1 is None:
        C.S.op('dve', lambda e: e.tensor_scalar(out=out, in0=in0, scalar1=s1, scalar2=None, op0=op0),
               reads=reads, writes=writes)
    else:
        C.S.op('dve', lambda e: e.tensor_scalar(out=out, in0=in0, scalar1=s1, scalar2=s2, op0=op0, op1=op1),
               reads=reads, writes=writes)


def cp(C, eng, out, in_, reads, writes):
    if eng == 'act':
        C.S.op('act', lambda e: e.copy(out=out, in_=in_), reads=reads, writes=writes)
    else:
        C.S.op(eng, lambda e: e.tensor_copy(out=out, in_=in_), reads=reads, writes=writes)


def rms_rstd(C, x_ap, x_res, width, junk, junk_res, ss, rstd, tag):
    act(C, junk, x_ap, AF.Square, [x_res], [junk_res, tag + 'ss'], accum_out=ss)
    act(C, ss, ss, AF.Sqrt, [tag + 'ss'], [tag + 'ss'], scale=1.0 / width, bias=EPS)
    C.S.op('dve', lambda e: e.reciprocal(out=rstd, in_=ss), reads=[tag + 'ss'], writes=[tag + 'rstd'])


def norm_T(C, x_ap, x_res, gain, gain_res, xn, xn_res, W, outT, outT_res, tile_sl, out_dt_copy='act'):
    W_ = W
    rms_rstd(C, x_ap, x_res, D, W_['junk'][:], 'junk', W_['ss'][:], W_['rstd'][:], 'nt_')
    stt(C, xn, x_ap, W_['rstd'][:, 0:1], gain, ALU.mult, ALU.mult, [x_res, 'nt_rstd', gain_res], [xn_res])
    transpose_to(C, xn, xn_res, KT, outT, outT_res, tile_sl)


def transpose_to(C, src, src_res, nk, outT, outT_res, tile_sl, interleave=False):
    srcv = src.rearrange("r (p k) -> r k p", k=nk) if interleave else None
    for k4 in range(0, nk, 4):
        p, pn = C.nb()
        n = min(4, nk - k4)
        for j in range(n):
            k = k4 + j
            sin = srcv[:, k, :] if interleave else src[:, k * 128:(k + 1) * 128]
            C.S.op('pe', lambda e: e.transpose(p[:, j * 128:(j + 1) * 128], sin, C.ident[:]),
                   reads=[src_res, 'ident'], writes=[pn])
        eng = 'act' if (k4 // 4) % 2 == 0 else 'dve'
        cp(C, eng, outT[:, k4:k4 + n, tile_sl], p[:, 0:n * 128].rearrange("p (k t) -> p k t", k=n),
           [pn], [outT_res])


def load_w_block(C, wbuf, wres, src_ap, nk, ncol):
    C.S.dma('pool', wbuf[:, 0:nk, 0:ncol], src_ap.rearrange("(k p) c -> p k c", p=128), writes=[wres])


def convert_weights(C, spec, bg=False):
    for item in spec:
        nm, nblk, ncol = item[0], item[1], item[3]
        src = C.I[WSRC[nm]]
        for blk in range(nblk):
            sap = src[:, blk * ncol:(blk + 1) * ncol].rearrange("(k p) c -> p k c", p=128)
            if bg:
                C.S.dma_bg(C.Z['W_' + nm][blk], sap, writes=[('Wc', nm, blk)])
            else:
                C.S.dma('pool', C.Z['W_' + nm][blk], sap, writes=[('Wc', nm, blk)])


def load_w_tiled(C, wbuf, wres, nm, blk, nk, ncol):
    C.S.dma('pool', wbuf[:, 0:nk, 0:ncol], C.Z['W_' + nm][blk], reads=[('Wc', nm, blk)], writes=[wres])


def gate_prep(C, f, c, own, dirs, lhs_fn, lhs_res_fn, rw, ri, W, ktok, ktok_res, qT, kT, tsl, filler=None):
    S = C.S
    Z = C.Z
    gs = (1.0 / 16.0) if f == 0 else 1.0
    lnk = 0.0 if f == 0 else LN_KS
    lnq = LN_QS if f == 0 else 0.0
    N = {d: dict(zip(('sp', 'isb', 'dec', 'kd', 'eb', 'enb', 'qt', 'kt'),
                     ['%s%d%s' % (n_, d, ('_%d' % (c % 2)) if n_ in ('kd', 'qt', 'kt') else '') for n_ in ('sp', 'isb', 'dec', 'kd', 'eb', 'enb', 'qt', 'kt')])) for d in dirs}
    for d in dirs:
        z, zn = C.nb()
        mm(C, z[:, :], lhs_fn(d), rw[0:17, d, :], True, True, [lhs_res_fn(d), 'rw%d' % f], [zn])
        sp = W['sp'][d]
        act(C, sp[:], z[:, :], AF.Exp, [zn], [N[d]['sp']], scale=-1.0)
        act(C, sp[:], sp[:], AF.Ln, [N[d]['sp']], [N[d]['sp']], bias=1.0, scale=1.0)
        if f == 1:
            zi, zin = C.nb()
            mm(C, zi[:, :], lhs_fn(d), ri[0:17, d, :], True, True, [lhs_res_fn(d), 'ri'], [zin])
            cp(C, 'dve', W['isb'][d][:], zi[:, :], [zin], [N[d]['isb']])
    if filler is not None:
        filler()
    P2 = {}
    for d in dirs:
        sp = W['sp'][d]
        isb = W['isb'][d]
        SPN, ISN = N[d]['sp'], N[d]['isb']
        dp, dpn = C.nb()
        mm(C, dp[:, :], C.ustr[:, d, :], sp[:], True, f == 0, ['ustr', SPN], [dpn])
        if f == 1:
            mm(C, dp[:, :], C.negi[:], isb[:], False, True, ['negi', ISN], [dpn])
        cs, csn = C.nb()
        for h in range(4):
            mm(C, cs[:, h * 128:(h + 1) * 128], sp[:, h * 128:(h + 1) * 128], C.mincl[:, d, :], True, True,
               [SPN, 'mincl'], [csn])
        ci, cin = None, None
        if own and f == 1:
            ci, cin = C.nb()
            for h in range(4):
                mm(C, ci[:, h * 128:(h + 1) * 128], sp[:, h * 128:(h + 1) * 128], C.mincl[:, d, :], True, False,
                   [SPN, 'mincl'], [cin])
                mm(C, ci[:, h * 128:(h + 1) * 128], isb[:, h * 128:(h + 1) * 128], C.ident[:], False, True,
                   [ISN, 'ident'], [cin])
        P2[d] = (dp, dpn, cs, csn, ci, cin)
    for d in dirs:
        dp, dpn, cs, csn, ci, cin = P2[d]
        DCN, KDN, EBN, ENN, QTN, KTN = [N[d][k_] for k_ in ('dec', 'kd', 'eb', 'enb', 'qt', 'kt')]
        dec = W['dec'][d]
        act(C, dec[:], dp[:, :], AF.Exp, [dpn], [DCN], scale=-gs, bias=lnk)
        kd = W['kd%d' % f][d][c % 2]
        if f == 0:
            tt(C, kd[:], ktok[:], dec[:], ALU.mult, [ktok_res, DCN], [KDN])
        else:
            kv = ktok[:].rearrange("p (h k d) -> p h k d", h=4, k=2)
            kdv = kd[:].rearrange("p (h k d) -> p h k d", h=4, k=2)
            dv_ = dec[:].rearrange("p (h d) -> p h d", h=4)
            for k in range(2):
                tt(C, kdv[:, :, k, :], kv[:, :, k, :], dv_, ALU.mult, [ktok_res, DCN], [KDN])
        S.dma('sp', Z['KD%d' % f][d, c], kd[:], reads=[KDN], writes=[('KD', f, d, c)])
        col = 127 if d == 0 else 0
        csv = cs[:, :].rearrange("p (h t) -> p h t", h=4)
        act(C, C.elast[f][:, d, c, :], csv[:, :, col], AF.Exp, [csn], [('elast', f)], scale=-gs)
        if own:
            eb = W['eb'][d]
            enb = W['enb'][d]
            act(C, eb[:], cs[:, :], AF.Exp, [csn], [EBN], scale=-gs, bias=lnq)
            if f == 0:
                act(C, enb[:], cs[:, :], AF.Exp, [csn], [ENN], scale=gs)
            else:
                act(C, enb[:], ci[:, :], AF.Exp, [cin], [ENN], scale=1.0, bias=lnk)
            qt = W['qt%d' % f][d][c % 2]
            kt_ = W['kt%d' % f][d][c % 2]
            ebv = eb[:].rearrange("p (h t) -> p h t", h=4)
            enbv = enb[:].rearrange("p (h t) -> p h t", h=4)
            if f == 0:
                tt(C, qt[:].rearrange("p (h t) -> p h t", h=4), qT[:, :, tsl], ebv, ALU.mult, ['qT', EBN], [QTN])
                tt(C, kt_[:].rearrange("p (h t) -> p h t", h=4), kT[:, :, tsl], enbv, ALU.mult, ['kT', ENN], [KTN])
            else:
                qtv = qt[:].rearrange("p (h k t) -> p h k t", h=4, k=2)
                ktv = kt_[:].rearrange("p (h k t) -> p h k t", h=4, k=2)
                qTv = qT[:].rearrange("p (h k) t -> p h k t", h=4)
                kTv = kT[:].rearrange("p (h k) t -> p h k t", h=4)
                for k in range(2):
                    tt(C, qtv[:, :, k, :], qTv[:, :, k, tsl], ebv, ALU.mult, ['qT', EBN], [QTN])
                    tt(C, ktv[:, :, k, :], kTv[:, :, k, tsl], enbv, ALU.mult, ['kT', ENN], [KTN])
            S.dma('sp', Z['QT%d' % f][d, c], qt[:], reads=[QTN], writes=[('QT', f, d, c)])
            S.dma('sp', Z['KT%d' % f][d, c], kt_[:], reads=[KTN], writes=[('KT', f, d, c)])


def alloc_gate_work(C, es2, W, pfx=''):
    S = C.S

    def sbt(name, shape, dt):
        return es2.enter_context(C.nc.sbuf_tensor(pfx + name, shape, dt))
    for nm_ in ('sp', 'isb', 'dec', 'eb', 'enb'):
        W[nm_] = [sbt("g_%s%d" % (nm_, d), [128, 512], F32) for d in range(2)]
    return sbt


def stage_A(C, stop_after):
    S = C.S
    I = C.I
    Z = C.Z
    nc = C.nc
    with ExitStack() as es2:
        W = {}
        sbt = alloc_gate_work(C, es2, W)
        W['junk'] = sbt("a_junk", [128, D], BF16)
        W['ss'] = sbt("a_ss", [128, 1], F32)
        W['rstd'] = sbt("a_rstd", [128, 1], F32)
        gain = sbt("a_gain", [128, D], F32)
        xbuf = [sbt("a_x%d" % i, [128, D], F32) for i in range(2)]
        xn = sbt("a_xn", [128, D], F32)
        xnT = sbt("a_xnT", [128, KT, 512], BF16)
        wbuf = [sbt("a_w%d" % i, [128, KT, 512], BF16) for i in range(3)]
        wsm = sbt("a_wsm", [128, KT, 48], BF16)
        qT = sbt("a_qT", [128, 4, 512], F32)
        kT = sbt("a_kT", [128, 4, 512], F32)
        stg = [sbt("a_stg%d" % i, [128, 512], F32) for i in range(2)]
        ktok = sbt("a_ktok", [128, 512], F32)
        vtok = sbt("a_vtok", [128, 1024], BF16)
        lr1T = sbt("a_lr1T", [17, 2, 512], F32)
        mgs = sbt("a_mgs", [16, 512], F32)
        rw = sbt("a_rw", [17, 2, 512], F32)
        zt = sbt("a_zero", [128, 8, 2], F32)
        W['kd0'] = [[sbt("a_kd%d%d" % (d, i), [128, 512], BF16) for i in range(2)] for d in range(2)]
        W['qt0'] = [[sbt("a_qt%d%d" % (d, i), [128, 512], BF16) for i in range(2)] for d in range(2)]
        W['kt0'] = [[sbt("a_kt%d%d" % (d, i), [128, 512], BF16) for i in range(2)] for d in range(2)]

        S.dma('sp', gain[:], I['g_mix'], writes=['gain'])
        for d in range(2):
            S.dma('sp', rw[:, d, :], I['wlr1'][d], writes=['rw0'])
        S.op('dve', lambda e: e.memset(lr1T[:], 1.0), writes=['lr1T0', 'lr1T1'])
        S.op('dve', lambda e: e.memset(zt[:], 0.0), writes=['zt'])
        S.dma('sp', Z['mxT'][:, :, 0:2].rearrange("c p t -> p c t"), zt[:], reads=['zt'], writes=[('mxT', 'h0')])
        S.dma('sp', Z['mxT'][:, :, SEQ + 2:SEQ + 4].rearrange("c p t -> p c t"), zt[:], reads=['zt'], writes=[('mxT', 'h1')])
        load_w_block(C, wsm, 'wsm', I['w_sm'], KT, 48)
        convert_weights(C, WCONV_A)

        wi = [0]

        def next_w(nm, blk):
            i = wi[0]
            wi[0] = (i + 1) % 3
            load_w_tiled(C, wbuf[i], 'w%d' % i, nm, blk, KT, 512)
            return wbuf[i], 'w%d' % i

        si = [0]

        def next_stg():
            i = si[0]
            si[0] = (i + 1) % 2
            return stg[i], 'stg%d' % i

        def fm_block(w, wres, ncol_tiles, M, evac):
            for ct in range(ncol_tiles):
                p, pn = C.nb()
                for k in range(KT):
                    mm(C, p[0:M, :], w[:, k, ct * 128:ct * 128 + M], xnT[:, k, :], k == 0, k == KT - 1,
                       [wres, 'xnT'], [pn])
                evac(ct, p, pn)

        for g in range(8):
            own = g < 4
            t0 = g * 512
            for j in range(4):
                t = g * 4 + j
                xb = xbuf[t % 2]
                xr = 'x%d' % (t % 2)
                if not (g > 0 and j < 2):
                    S.dma('sp', xb[:], I['x'][t * 128:(t + 1) * 128, :], writes=[xr])
                norm_T(C, xb[:], xr, gain[:], 'gain', xn[:], 'xn', W, xnT, 'xnT', slice(j * 128, (j + 1) * 128))
            if g + 1 < 8:
                for j in range(2):
                    t = (g + 1) * 4 + j
                    S.dma('sp', xbuf[t % 2][:], I['x'][t * 128:(t + 1) * 128, :], writes=['x%d' % (t % 2)])
            for sidx in range(3):
                if sidx == 0 and not own:
                    continue
                p, pn = C.nb()
                for k in range(KT):
                    mm(C, p[0:16, :], wsm[:, k, sidx * 16:(sidx + 1) * 16], xnT[:, k, :], k == 0, k == KT - 1,
                       ['wsm', 'xnT'], [pn])
                if sidx < 2:
                    cp(C, 'act', lr1T[0:16, sidx, :], p[0:16, :], [pn], ['lr1T%d' % sidx])
                else:
                    cp(C, 'act', mgs[:], p[0:16, :], [pn], ['mgs'])
                    S.dma('sp', Z['mgT'][:, t0:t0 + 512], mgs[:], reads=['mgs'], writes=[('mgT', g)])
            if own:
                w, wr = next_w('q', 0)
                fm_block(w, wr, 4, 128, lambda ct, p, pn: cp(C, 'act', qT[:, ct, :], p[:, :], [pn], ['qT']))
            w, wr = next_w('k', 0)
            if own:
                fm_block(w, wr, 4, 128, lambda ct, p, pn: cp(C, 'dve', kT[:, ct, :], p[:, :], [pn], ['kT']))
            wk, wkr = w, wr
            wv_ = [next_w('v', vb) for vb in range(2)]
            for j in range(4):
                c = g * 4 + j
                tsl = slice(j * 128, (j + 1) * 128)
                p, pn = C.nb()
                for k in range(KT):
                    mm(C, p[:, :], xnT[:, k, tsl], wk[:, k, :], k == 0, k == KT - 1, ['xnT', wkr], [pn])
                cp(C, 'act', ktok[:], p[:, :], [pn], ['ktok'])

                def v_filler(j=j, c=c, tsl=tsl):
                    for vb in range(2):
                        w, wr = wv_[vb]
                        p, pn = C.nb()
                        for k in range(KT):
                            mm(C, p[:, :], xnT[:, k, tsl], w[:, k, :], k == 0, k == KT - 1, ['xnT', wr], [pn])
                        cp(C, 'act' if vb == 0 else 'dve', vtok[:, vb * 512:(vb + 1) * 512], p[:, :], [pn], [('vtok', vb)])
                        S.dma('sp', Z['V0'][c, :, vb * 512:(vb + 1) * 512], vtok[:, vb * 512:(vb + 1) * 512],
                              reads=[('vtok', vb)], writes=[('V0', c, vb)])
                gate_prep(C, 0, c, own, (0, 1) if own else (1,),
                          lambda d: lr1T[0:17, d, tsl], lambda d: 'lr1T%d' % d, rw, None, W,
                          ktok, 'ktok', qT, kT, tsl, filler=v_filler)
            for mb in range(2):
                w, wr = next_w('mx', mb)

                def ev_mx(ct, p, pn, mb=mb):
                    s_, sr = next_stg()
                    cp(C, 'act' if ct % 2 == 0 else 'dve', s_[:], p[:, :], [pn], [sr])
                    S.dma('sp', Z['mxT'][mb * 4 + ct, :, 2 + t0:2 + t0 + 512], s_[:], reads=[sr],
                          writes=[('mxT', g, mb, ct)])
                fm_block(w, wr, 4, 128, ev_mx)


def _consts():
    c = {}
    ii = np.arange(128)
    c['c_ident'] = np.eye(128, dtype=np.float32)
    c['c_negi'] = np.where(np.eye(128) > 0, np.float32(-1.0), np.float32(0.0)).astype(np.float32)
    c['c_ones'] = np.ones((128, 128), np.float32)
    s = ii[:, None]
    t = ii[None, :]
    c['c_mincl'] = np.stack([(s <= t), (s >= t)]).astype(np.float32)
    c['c_ustr'] = np.stack([(s > t), (s < t)]).astype(np.float32)
    c['c_iota32'] = np.tile(np.arange(32, dtype=np.float32)[None, :], (128, 1))
    c['c_brow'] = np.tile(np.arange(64, dtype=np.float32)[None, :] * np.float32(128.0), (32, 1)).astype(np.float32)
    c['c_jrow'] = np.tile(np.arange(4, dtype=np.float32)[None, :], (128, 1))
    c['c_zero'] = np.zeros((128, D), np.float32)
    return c


def _rep(v, n=128):
    return np.ascontiguousarray(np.broadcast_to(np.asarray(v, np.float32).reshape(1, -1), (n, v.size)))


def _prep_shared(inp):
    w_in = np.asarray(inp['w_in'][0], np.float32)
    offs = np.cumsum([0, 512, 512, 1024, 1024, 16, 16, 1024, 1024, 16, 2048, 2048])
    gq, gk, gv, gg, lrf, lrb, mx, mz, mg, ga, gb = [w_in[:, offs[i]:offs[i + 1]] for i in range(11)]
    base = {}
    base['w_q'] = np.ascontiguousarray(gq)
    base['w_k'] = np.ascontiguousarray(gk)
    base['w_v'] = np.ascontiguousarray(gv)
    base['w_mx'] = np.ascontiguousarray(mx)
    base['w_gz'] = np.ascontiguousarray(np.concatenate([gg, mz], axis=1))
    base['w_gab'] = np.ascontiguousarray(np.concatenate([ga, gb], axis=1))
    for nm, key in (('g_mix', 'norm_mix'), ('g_xattn', 'norm_xattn'), ('g_mem', 'norm_mem'),
                    ('g_ffn', 'norm_ffn'), ('g_gla', 'gla_norm'), ('g_m', 'm_norm')):
        base[nm] = _rep(np.asarray(inp[key][0], np.float32))
    base['g_final'] = _rep(np.asarray(inp['norm_final'], np.float32))
    for nm in ('m_wq', 'm_wk', 'm_wv'):
        base[nm] = np.ascontiguousarray(np.asarray(inp[nm][0], np.float32))
    base['w_ba'] = np.ascontiguousarray(inp['w_branch_a'][0])
    base['w_bb'] = np.ascontiguousarray(inp['w_branch_b'][0])
    base['w_mix'] = np.ascontiguousarray(inp['w_mix_out'][0])
    base['w_xq'] = np.ascontiguousarray(inp['w_xq'][0])
    base['w_xkv'] = np.ascontiguousarray(inp['w_xkv'][0])
    base['w_xo'] = np.ascontiguousarray(inp['w_xo'][0])
    wr = np.asarray(inp['w_router'][0], np.float32)
    base['w_rt'] = np.ascontiguousarray(np.concatenate([np.asarray(inp['w_group'][0], np.float32)] +
                                                       [wr[g] for g in range(4)], axis=1))
    base['b_rt'] = _rep(np.concatenate([np.asarray(inp['b_group'][0], np.float32),
                                        np.asarray(inp['b_router'][0], np.float32).reshape(-1)]))
    base['w_gate'] = np.ascontiguousarray(inp['w_gate'][0])
    base['w_up'] = np.ascontiguousarray(inp['w_up'][0])
    base['w_down'] = np.ascontiguousarray(inp['w_down'][0])
    base['conv_b'] = np.ascontiguousarray(np.asarray(inp['conv_b'][0], np.float32).reshape(8, 128).T)
    base.update(_consts())
    res = []
    cw = np.asarray(inp['conv_w'][0], np.float32)[:, 0, :]
    gbias = np.asarray(inp['m_gate_bias'][0], np.float32)
    wlr = [np.asarray(inp['gla_w_lr_f'][0], np.float32), np.asarray(inp['gla_w_lr_b'][0], np.float32)]
    blr = [np.asarray(inp['gla_b_lr_f'][0], np.float32), np.asarray(inp['gla_b_lr_b'][0], np.float32)]
    lrs = [lrf, lrb]
    for half in range(2):
        dct = dict(base)
        o = [0, 1] if half == 0 else [1, 0]
        mgl = np.concatenate([mg[:, 8 * o[0]:8 * o[0] + 8], mg[:, 8 * o[1]:8 * o[1] + 8]], axis=1)
        dct['w_sm'] = np.ascontiguousarray(np.concatenate([lrs[o[0]], lrs[o[1]], mgl], axis=1))
        wl = np.zeros((2, 17, 512), np.float32)
        rf = np.zeros((2, 17, 512), np.float32)
        ri = np.zeros((2, 17, 512), np.float32)
        for d in range(2):
            wl[d, 0:16] = wlr[o[d]]
            wl[d, 16] = blr[o[d]]
            for h in range(4):
                ri[d, 8 * d + h, h * 128:(h + 1) * 128] = 1.0
                ri[d, 16, h * 128:(h + 1) * 128] = gbias[2 * o[d], h]
                rf[d, 8 * d + 4 + h, h * 128:(h + 1) * 128] = 1.0
                rf[d, 16, h * 128:(h + 1) * 128] = gbias[2 * o[d] + 1, h]
        dct['wlr1'] = wl
        dct['rf1'] = rf
        dct['ri1'] = ri
        cwl = cw if half == 0 else cw[::-1]
        dct['conv_w'] = np.ascontiguousarray(cwl.T.reshape(8, 128, 5).transpose(1, 0, 2))
        res.append(dct)
    return res


def _in_maps(inp):
    shared = _prep_shared(inp)
    x = np.asarray(inp['x'], np.float32)
    mem = np.asarray(inp['mem'], np.float32)
    maps = []
    for core in range(8):
        b, half = core // 2, core % 2
        m = dict(shared[half])
        xs = x[b] if half == 0 else x[b][::-1]
        m['x'] = np.ascontiguousarray(xs)
        m['mem'] = np.ascontiguousarray(mem[b])
        maps.append(m)
    return maps


def kernel(**inputs):
    nc = build_program()
    maps = _in_maps(inputs)
    res = run_bass_kernel_spmd(nc, maps, core_ids=list(range(8)))
    out = np.zeros((NB, SEQ, D), np.float32)
    for core in range(8):
        b, half = core // 2, core % 2
        o = np.asarray(res.results[core]['out'], np.float32)
        if half == 0:
            out[b, 0:OWN] = o
        else:
            out[b, OWN:] = o[::-1]
    return out


def stage_B(C):
    S = C.S
    I = C.I
    Z = C.Z
    with ExitStack() as es2:
        W = {}
        sbt = alloc_gate_work(C, es2, W, 'B')
        mxg2 = [sbt("b_mxg%d" % i, [128, 8, 516], F32) for i in range(2)]
        xcT = sbt("b_xcT", [128, 8, 512], BF16)
        mxgb = sbt("b_mxgb", [128, 8, 516], BF16)
        dg = sbt("b_dg", [128, 8, 5, 128], BF16)
        cw = sbt("b_cw", [128, 8, 5], F32)
        cb = sbt("b_cb", [128, 8], F32)
        wq = sbt("b_wq", [128, 4, 2, 256], BF16)
        wk = sbt("b_wk", [128, 4, 2, 256], BF16)
        wv = sbt("b_wv", [128, 4, 2, 256], BF16)
        qT = sbt("b_qT", [128, 8, 512], F32)
        kT = sbt("b_kT", [128, 8, 512], F32)
        ktok = sbt("b_ktok", [128, 1024], F32)
        vp2 = [sbt("b_vp%d" % i, [128, 4, 257], BF16) for i in range(2)]
        mg1T2 = [sbt("b_mg1T%d" % i, [17, 512], F32) for i in range(2)]
        rf = sbt("b_rf", [17, 2, 512], F32)
        ri = sbt("b_ri", [17, 2, 512], F32)
        W['kd1'] = [[sbt("b_kd%d%d" % (d, i), [128, 1024], BF16) for i in range(2)] for d in range(2)]
        W['qt1'] = [[sbt("b_qt%d%d" % (d, i), [128, 1024], BF16) for i in range(2)] for d in range(2)]
        W['kt1'] = [[sbt("b_kt%d%d" % (d, i), [128, 1024], BF16) for i in range(2)] for d in range(2)]

        S.dma('sp', cw[:], I['conv_w'], writes=['cw'])
        S.dma('sp', cb[:], I['conv_b'], writes=['cb'])
        for d in range(2):
            S.dma('sp', rf[:, d, :], I['rf1'][d], writes=['rw1'])
            S.dma('sp', ri[:, d, :], I['ri1'][d], writes=['ri'])
        for wt, nm in ((wq, 'm_wq'), (wk, 'm_wk'), (wv, 'm_wv')):
            for h in range(4):
                S.dma('pool', wt[:, h, :, :], I[nm][h].rearrange("(k p) e -> p k e", p=128), writes=['b_' + nm])
        for i in range(2):
            S.op('dve', lambda e: e.memset(mg1T2[i][:], 1.0), writes=['mg1T%d' % i])
            S.op('dve', lambda e: e.memset(vp2[i][:], 1.0), writes=['vp%d' % i])
        for ct in range(8):
            for j in range(5):
                ts(C, dg[:, ct, j, :], C.ident[:], cw[:, ct, j:j + 1], None, ALU.mult, None, ['ident', 'cw'], ['dg'])
        convert_weights(C, WCONV_POST, bg=True)
        for b in range(NBLK):
            S.dma_bg(Z['XR'][b * 128:(b + 1) * 128, :], I['c_zero'], writes=[('XRz', b)])

        def b_loads(g):
            t0 = g * 512
            S.dma('sp', mxg2[g % 2][:], Z['mxT'][:, :, t0:t0 + 516].rearrange("c p t -> p c t"), writes=['mxg%d' % (g % 2)])
            S.dma('sp', mg1T2[g % 2][0:16, :], Z['mgT'][:, t0:t0 + 512], writes=['mg1T%d' % (g % 2)])

        b_loads(0)
        for g in range(8):
            own = g < 4
            t0 = g * 512
            mxg, MXGN = mxg2[g % 2], 'mxg%d' % (g % 2)
            mg1T, MGN = mg1T2[g % 2], 'mg1T%d' % (g % 2)
            if g + 1 < 8:
                b_loads(g + 1)
            cp(C, 'act', mxgb[:], mxg[:], [MXGN], ['mxb'])
            for ct in range(8):
                p, pn = C.nb()
                for j in range(5):
                    mm(C, p[:, :], dg[:, ct, j, :], mxgb[:, ct, j:j + 512], j == 0, j == 4, ['dg', 'mxb'], [pn])
                act(C, xcT[:, ct, :], p[:, :], AF.Silu, [pn, 'cb'], ['xcT'], bias=cb[:, ct:ct + 1], scale=1.0)
            if own:
                for (wt, wn, dst, dn) in ((wq, 'b_m_wq', qT, 'qT'), (wk, 'b_m_wk', kT, 'kT')):
                    for h in range(4):
                        for et in range(2):
                            p, pn = C.nb()
                            for dt_ in range(2):
                                mm(C, p[:, :], wt[:, h, dt_, et * 128:(et + 1) * 128], xcT[:, 2 * h + dt_, :],
                                   dt_ == 0, dt_ == 1, [wn, 'xcT'], [pn])
                            cp(C, 'act' if et == 0 else 'dve', dst[:, h * 2 + et, :], p[:, :], [pn], [dn])
            for j in range(4):
                c = g * 4 + j
                tsl = slice(j * 128, (j + 1) * 128)
                vp, VPN = vp2[c % 2], 'vp%d' % (c % 2)
                for hp in range(2):
                    p, pn = C.nb()
                    p2, pn2 = C.nb()
                    for h2 in range(2):
                        h = hp * 2 + h2
                        for dt_ in range(2):
                            mm(C, p[:, h2 * 256:(h2 + 1) * 256], xcT[:, 2 * h + dt_, tsl], wk[:, h, dt_, :],
                               dt_ == 0, dt_ == 1, ['xcT', 'b_m_wk'], [pn])
                        for dt_ in range(2):
                            mm(C, p2[:, h2 * 256:(h2 + 1) * 256], mxgb[:, 2 * h + dt_, 2 + j * 128:2 + (j + 1) * 128], wv[:, h, dt_, :],
                               dt_ == 0, dt_ == 1, ['mxb', 'b_m_wv'], [pn2])
                    cp(C, 'act', ktok[:, hp * 512:(hp + 1) * 512], p[:, :], [pn], ['ktok'])
                    cp(C, 'dve', vp[:, hp * 2:hp * 2 + 2, 0:256], p2[:, :].rearrange("p (h e) -> p h e", h=2),
                       [pn2], [VPN])
                S.dma('sp', Z['V1'][c], vp[:].rearrange("p h e -> p (h e)"), reads=[VPN], writes=[('V1', c)])
                gate_prep(C, 1, c, own, (0, 1) if own else (1,),
                          lambda d: mg1T[0:17, tsl], lambda d: MGN, rf, ri, W,
                          ktok, 'ktok', qT, kT, tsl)


def stage_scan(C, f):
    S = C.S
    I = C.I
    Z = C.Z
    nkt = 1 if f == 0 else 2
    dv = 256 if f == 0 else 257
    hk = 4 * nkt * 128
    with ExitStack() as es2:
        def sbt(name, shape, dt):
            return es2.enter_context(C.nc.sbuf_tensor("s%d_%s" % (f, name), shape, dt))
        St = sbt("St", [128, 2, 4, nkt, dv], F32)
        Sb = sbt("Sb", [128, 2, 4, nkt, dv], BF16)
        OF = sbt("OF", [128, 16, 4, 256], F32)
        qtb = [[sbt("qt%d%d" % (d, i), [128, hk], BF16) for i in range(2)] for d in range(2)]
        ktb = [[sbt("kt%d%d" % (d, i), [128, hk], BF16) for i in range(2)] for d in range(2)]
        kdb = [[sbt("kd%d%d" % (d, i), [128, hk], BF16) for i in range(2)] for d in range(2)]
        vb = [[sbt("v%d%d" % (d, i), [128, 4 * dv], BF16) for i in range(2)] for d in range(2)]
        sTb = [sbt("sT%d" % i, [128, 128], BF16) for i in range(4)]
        gn = sbt("gn", [128, 1024], F32)
        yn = sbt("yn", [128, 4, 256], F32)
        tot4 = sbt("tot", [128, 4, 256], F32)
        junk4 = sbt("junk", [128, 4, 256], BF16)
        ss4 = sbt("ss", [128, 4], F32)
        rstd4 = sbt("rstd", [128, 4], F32)
        rr8 = sbt("rr", [128, 8], F32)
        S.dma('sp', gn[:], I['g_gla'] if f == 0 else I['g_m'], writes=['gn'])
        S.op('dve', lambda e: e.memset(St[:], 0.0),
             writes=[('St', d, h, k) for d in range(2) for h in range(4) for k in range(nkt)])
        S.op('dve', lambda e: e.memset(Sb[:], 0.0), writes=[('Sb', d, h) for d in range(2) for h in range(4)])
        cnt = [0, 0]
        sti = [0]

        cnt_l = [0, 0]

        def chunk_loads(d, c, full):
            i = cnt_l[d] % 2
            cnt_l[d] += 1
            tg = "%d%d" % (d, i)
            qt, kt_, kd, v = qtb[d][i], ktb[d][i], kdb[d][i], vb[d][i]
            if full:
                S.dma('sp', qt[:], Z['QT%d' % f][d, c], writes=['qt' + tg])
                S.dma('sp', kt_[:], Z['KT%d' % f][d, c], writes=['kt' + tg])
            S.dma('sp', kd[:], Z['KD%d' % f][d, c], writes=['kd' + tg])
            S.dma('sp', v[:], Z['V%d' % f][c], writes=['v' + tg])

        def chunk_step(d, c, full, last):
            i = cnt[d] % 2
            cnt[d] += 1
            tg = "%d%d" % (d, i)
            qt, kt_, kd, v = qtb[d][i], ktb[d][i], kdb[d][i], vb[d][i]
            hs = range(4)
            vhs = [v[:, h * dv:(h + 1) * dv] for h in hs]
            if full:
                os_, ons, ss_, sns = [], [], [], []
                for h in hs:
                    o, on = C.nb()
                    s, sn = C.nb()
                    for k in range(nkt):
                        qs = slice((h * nkt + k) * 128, (h * nkt + k + 1) * 128)
                        mm(C, o[:, 0:dv], qt[:, qs], Sb[:, d, h, k, :], k == 0, False, ['qt' + tg, ('Sb', d, h)], [on])
                    for k in range(nkt):
                        qs = slice((h * nkt + k) * 128, (h * nkt + k + 1) * 128)
                        mm(C, s[:, 0:128], kt_[:, qs], qt[:, qs], k == 0, k == nkt - 1, ['kt' + tg, 'qt' + tg], [sn])
                    os_.append(o)
                    ons.append(on)
                    ss_.append(s)
                    sns.append(sn)
                for h in hs:
                    tt(C, sTb[h][:], ss_[h][:, 0:128], C.maskb[:, d, :], ALU.mult, [sns[h], 'maskb'], ['sT%d' % h])
                for h in hs:
                    mm(C, os_[h][:, 0:dv], sTb[h][:], vhs[h], False, True, ['sT%d' % h, 'v' + tg], [ons[h]])
                for h in hs:
                    o, on = os_[h], ons[h]
                    tot = tot4[:, h, :]
                    junk = junk4[:, h, :]
                    ss = ss4[:, h:h + 1]
                    rstd = rstd4[:, h:h + 1]
                    rr = rr8[:, d * 4 + h:d * 4 + h + 1]
                    RRN = 'rr%d%d' % (d, h)
                    TOTN = 'tot%d' % h
                    if f == 1:
                        ts(C, rr, o[:, 256:257], -1.0, 1.0, ALU.mult, ALU.max, [on], [RRN])
                        tt(C, rr, rr, o[:, 256:257], ALU.max, [RRN, on], [RRN])
                        S.op('dve', lambda e: e.reciprocal(out=rr, in_=rr), reads=[RRN], writes=[RRN])
                    if d == 0:
                        if f == 0:
                            cp(C, 'act', OF[:, c, h, :], o[:, 0:256], [on], [('OF', c, h)])
                        else:
                            act(C, OF[:, c, h, :], o[:, 0:256], AF.Copy, [on, RRN], [('OF', c, h)], scale=rr)
                    else:
                        if f == 0:
                            tt(C, tot, OF[:, c, h, :], o[:, 0:256], ALU.add, [('OF', c, h), on], [TOTN])
                        else:
                            stt(C, tot, o[:, 0:256], rr, OF[:, c, h, :], ALU.mult, ALU.add,
                                [on, RRN, ('OF', c, h)], [TOTN])
                        rms_rstd(C, tot, TOTN, 256, junk, 'junk%d' % h, ss, rstd, 'sc%d_' % h)
                        stt(C, yn[:, h, :], tot, rstd, gn[:, h * 256:(h + 1) * 256], ALU.mult, ALU.mult,
                            [TOTN, 'sc%d_rstd' % h, 'gn'], [('yn', h)])
            if not last:
                us = []
                for h in hs:
                    for k in range(nkt):
                        u, un = C.nb()
                        ks = slice((h * nkt + k) * 128, (h * nkt + k + 1) * 128)
                        mm(C, u[:, 0:dv], kd[:, ks], vhs[h], True, True, ['kd' + tg, 'v' + tg], [un])
                        us.append((h, k, u, un))
                for (h, k, u, un) in us:
                    stt(C, St[:, d, h, k, :], St[:, d, h, k, :], C.elast[f][:, d, c, h:h + 1], u[:, 0:dv],
                        ALU.mult, ALU.add, [('St', d, h, k), ('elast', f), un], [('St', d, h, k)])
                    cp(C, 'act', Sb[:, d, h, k, :], St[:, d, h, k, :], [('St', d, h, k)], [('Sb', d, h)])
            if full and d == 1:
                S.dma('sp', Z['YN'][c, :, f * 1024:(f + 1) * 1024], yn[:].rearrange("p h e -> p (h e)"),
                      reads=[('yn', hh) for hh in range(4)], writes=[('YN', f, c)])

        steps = []
        for step in range(32):
            if step < 16:
                steps.append((0, step, True, step == 15))
            c1 = 31 - step
            steps.append((1, c1, c1 < 16, c1 == 0))
        per_dir = {0: [st for st in steps if st[0] == 0], 1: [st for st in steps if st[0] == 1]}
        done_l = [0, 0]
        done_c = [0, 0]
        for (d, c, full, last) in steps:
            while done_l[d] <= min(done_c[d] + 1, len(per_dir[d]) - 1):
                dd, cc, ff, _ = per_dir[d][done_l[d]]
                chunk_loads(dd, cc, ff)
                done_l[d] += 1
            chunk_step(d, c, full, last)
            done_c[d] += 1


def stage_post(C):
    S = C.S
    I = C.I
    Z = C.Z
    RT = C.RT
    with ExitStack() as es2:
        def sbt(name, shape, dt):
            return es2.enter_context(C.nc.sbuf_tensor("p_" + name, shape, dt))
        W = {}
        W['junk'] = sbt("junk", [128, D], BF16)
        W['ss'] = sbt("ss", [128, 1], F32)
        W['rstd'] = sbt("rstd", [128, 1], F32)
        gain = sbt("gain", [128, D], F32)
        xbuf = [sbt("x%d" % i, [128, D], F32) for i in range(2)]
        xn = sbt("xn", [128, D], F32)
        xnT = sbt("xnT", [128, KT, 512], BF16)
        big = sbt("big", [128, 4, D], F32)
        yT = sbt("yT", [128, KT, 512], BF16)
        mT = sbt("mT", [128, KT, 512], BF16)
        wall = sbt("wall", [128, 4, KT, 256], BF16)
        wbuf = [wall[:, i] for i in range(4)]
        wbig = [wall[:, 2 * j:2 * j + 2].rearrange("p a k c -> p (a k c)").rearrange("p (k c) -> p k c", c=512)
                for j in range(2)]
        kmT = sbt("kmT", [128, KT, 256], BF16)
        vm = sbt("vm", [128, 2, D], BF16)
        ynb = [sbt("ynb%d" % i, [128, 512], F32) for i in range(2)]
        gtmp = sbt("gtmp", [128, 512], F32)
        sga = sbt("sga", [128, 512], F32)
        sgb = sbt("sgb", [128, 512], F32)
        P4 = sbt("P", [128, 4, 256], F32)
        PT4 = sbt("PT", [128, 4, 2, 128], BF16)
        mxs4 = sbt("mxs", [128, 4], F32)
        rs4 = sbt("rs", [128, 4], F32)
        wrt = sbt("wrt", [128, KT, 36], F32)
        brt = sbt("brt", [128, 36], F32)
        jrow = sbt("jrow", [128, 4], F32)
        lg = sbt("lg", [128, 36], F32)
        sm = sbt("sm", [128, 16], F32)
        ge = sbt("ge", [128, 4], F32)
        ohg = sbt("ohg", [128, 4], F32)
        esel = sbt("esel", [128, 8], F32)
        top8 = sbt("top8", [128, 8], F32)
        idx8 = sbt("idx8", [128, 8], U32)
        if8 = sbt("if8", [128, 8], F32)

        S.dma('sp', wrt[:], I['w_rt'].rearrange("(k p) c -> p k c", p=128), writes=['wrt'])
        S.dma('sp', brt[:], I['b_rt'], writes=['brt'])
        S.dma('sp', jrow[:], I['c_jrow'], writes=['jrow'])

        wi = [0]

        def next_w(nm, blk, nk=KT):
            i = wi[0]
            wi[0] = (i + 1) % 4
            load_w_tiled(C, wbuf[i], 'w%d' % i, nm, blk, nk, 256)
            return wbuf[i], 'w%d' % i

        def next_w_direct(src_ap):
            i = wi[0]
            wi[0] = (i + 1) % 4
            load_w_block(C, wbuf[i], 'w%d' % i, src_ap, KT, 256)
            return wbuf[i], 'w%d' % i

        bi = [0]

        def next_wbig(nm, blk):
            j = bi[0]
            bi[0] = (j + 1) % 2
            names = ['w%d' % (2 * j), 'w%d' % (2 * j + 1)]
            C.S.dma('pool', wbig[j], C.Z['W_' + nm][blk], reads=[('Wc', nm, blk)], writes=names)
            return wbig[j], names

        yi = [0]

        def next_ynb():
            i = yi[0]
            yi[0] = (i + 1) % 2
            return ynb[i], 'ynb%d' % i

        S.dma('sp', gain[:], I['g_mem'], writes=['gain'])
        for mt in range(2):
            S.dma('sp', xbuf[mt][:], I['mem'][mt * 128:(mt + 1) * 128, :], writes=['x%d' % mt])
            norm_T(C, xbuf[mt][:], 'x%d' % mt, gain[:], 'gain', xn[:], 'xn', W, xnT, 'xnT',
                   slice(mt * 128, (mt + 1) * 128))
        for blk in range(8):
            w, wr = next_w_direct(I['w_xkv'][:, blk * 256:(blk + 1) * 256])
            for c2 in range(2):
                p, pn = C.nb()
                for k in range(KT):
                    mm(C, p[:, 0:256], w[:, k, c2 * 128:(c2 + 1) * 128], xnT[:, k, 0:256], k == 0, k == KT - 1,
                       [wr, 'xnT'], [pn])
                cp(C, 'act', kmT[:, blk * 2 + c2, :], p[:, 0:256], [pn], ['kmT'])
        for blk in range(8):
            w, wr = next_w_direct(I['w_xkv'][:, D + blk * 256:D + (blk + 1) * 256])
            for mt in range(2):
                p, pn = C.nb()
                for k in range(KT):
                    mm(C, p[:, 0:256], xnT[:, k, mt * 128:(mt + 1) * 128], w[:, k, :], k == 0, k == KT - 1,
                       ['xnT', wr], [pn])
                cp(C, 'dve', vm[:, mt, blk * 256:(blk + 1) * 256], p[:, 0:256], [pn], ['vm'])

        att_scale = float(512 ** -0.5)
        for g in range(4):
            tsls = [slice(j * 128, (j + 1) * 128) for j in range(4)]
            S.dma('sp', gain[:], I['g_mix'], writes=['gain'])
            for j in range(4):
                t = g * 4 + j
                xb, xr = xbuf[j % 2], 'x%d' % (j % 2)
                S.dma('sp', xb[:], I['x'][t * 128:(t + 1) * 128, :], writes=[xr])
                norm_T(C, xb[:], xr, gain[:], 'gain', xn[:], 'xn', W, xnT, 'xnT', tsls[j])
            for blk in range(4):
                w, wrs = next_wbig('gz', blk)
                cs_ = slice(blk * 512, (blk + 1) * 512)
                for j in range(4):
                    t = g * 4 + j
                    p, pn = C.nb()
                    for k in range(KT):
                        mm(C, p[:, :], xnT[:, k, tsls[j]], w[:, k, :], k == 0, k == KT - 1, ['xnT'] + wrs, [pn])
                    act(C, gtmp[:], p[:, :], AF.Silu if blk < 2 else AF.Sigmoid, [pn], ['gtmp'])
                    yb_, ybr = next_ynb()
                    S.dma('sp', yb_[:], Z['YN'][t, :, cs_], writes=[ybr])
                    tt(C, big[:, j, cs_], gtmp[:], yb_[:], ALU.mult, ['gtmp', ybr], [('big', j)])
            for j in range(4):
                transpose_to(C, big[:, j, :], ('big', j), KT, yT, 'yT', tsls[j])
            for cb in range(8):
                cs_ = slice(cb * 256, (cb + 1) * 256)
                wa, war = next_w('ba', cb, nk=8)
                wb_, wbr = next_w('bb', cb, nk=8)
                wga, wgar = next_w('gab', cb)
                wgb, wgbr = next_w('gab', 8 + cb)
                for c2 in range(2):
                    ct = cb * 2 + c2
                    c2s = slice(c2 * 128, (c2 + 1) * 128)
                    pa, pan = C.nb()
                    for k in range(8):
                        mm(C, pa[:, :], wa[:, k, c2s], yT[:, k, :], k == 0, k == 7, [war, 'yT'], [pan])
                    pb_, pbn = C.nb()
                    for k in range(8):
                        mm(C, pb_[:, :], wb_[:, k, c2s], yT[:, 8 + k, :], k == 0, k == 7, [wbr, 'yT'], [pbn])
                    ga, gan = C.nb()
                    for k in range(KT):
                        mm(C, ga[:, :], wga[:, k, c2s], xnT[:, k, :], k == 0, k == KT - 1, [wgar, 'xnT'], [gan])
                    gb, gbn = C.nb()
                    for k in range(KT):
                        mm(C, gb[:, :], wgb[:, k, c2s], xnT[:, k, :], k == 0, k == KT - 1, [wgbr, 'xnT'], [gbn])
                    act(C, sga[:], ga[:, :], AF.Sigmoid, [gan], ['sga'])
                    act(C, sgb[:], gb[:, :], AF.Sigmoid, [gbn], ['sgb'])
                    tt(C, sga[:], sga[:], pa[:, :], ALU.mult, ['sga', pan], ['sga'])
                    tt(C, sgb[:], sgb[:], pb_[:, :], ALU.mult, ['sgb', pbn], ['sgb'])
                    tt(C, mT[:, ct, :], sga[:], sgb[:], ALU.add, ['sga', 'sgb'], ['mT'])
            for cb in range(4):
                cs_ = slice(cb * 512, (cb + 1) * 512)
                w, wrs = next_wbig('mix', cb)
                for j in range(4):
                    t = g * 4 + j
                    p, pn = C.nb()
                    for k in range(KT):
                        mm(C, p[:, :], mT[:, k, tsls[j]], w[:, k, :], k == 0, k == KT - 1, ['mT'] + wrs, [pn])
                    yb_, ybr = next_ynb()
                    S.dma('sp', yb_[:], I['x'][t * 128:(t + 1) * 128, cs_], writes=[ybr])
                    tt(C, big[:, j, cs_], p[:, :], yb_[:], ALU.add, [pn, ybr], [('big', j)])
            S.dma('sp', gain[:], I['g_xattn'], writes=['gain'])
            for j in range(4):
                norm_T(C, big[:, j, :], ('big', j), gain[:], 'gain', xn[:], 'xn', W, xnT, 'xnT', tsls[j])
            for cb in range(8):
                w, wr = next_w('xq', cb)
                for c2 in range(2):
                    p, pn = C.nb()
                    for k in range(KT):
                        mm(C, p[:, :], w[:, k, c2 * 128:(c2 + 1) * 128], xnT[:, k, :], k == 0, k == KT - 1,
                           [wr, 'xnT'], [pn])
                    cp(C, 'act' if c2 == 0 else 'dve', yT[:, cb * 2 + c2, :], p[:, :], [pn], ['yT'])
            for j in range(4):
                ps_, pns = [], []
                for hd in range(4):
                    p, pn = C.nb()
                    ps_.append(p)
                    pns.append(pn)
                    for dt_ in range(4):
                        mm(C, p[:, 0:256], yT[:, hd * 4 + dt_, tsls[j]], kmT[:, hd * 4 + dt_, :], dt_ == 0, dt_ == 3,
                           ['yT', 'kmT'], [pn])
                for hd in range(4):
                    S.op('dve', lambda e: e.reduce_max(out=mxs4[:, hd:hd + 1], in_=ps_[hd][:, 0:256], axis=AX.X),
                         reads=[pns[hd]], writes=[('mxs', hd)])
                    ts(C, mxs4[:, hd:hd + 1], mxs4[:, hd:hd + 1], -att_scale, None, ALU.mult, None, [('mxs', hd)], [('mxs', hd)])
                for hd in range(4):
                    act(C, P4[:, hd, :], ps_[hd][:, 0:256], AF.Exp, [pns[hd], ('mxs', hd)], [('P', hd), ('rs', hd)],
                        scale=att_scale, bias=mxs4[:, hd:hd + 1], accum_out=rs4[:, hd:hd + 1])
                for hd in range(4):
                    pt_, ptn = C.nb()
                    for mt in range(2):
                        C.S.op('pe', lambda e: e.transpose(pt_[:, mt * 128:(mt + 1) * 128], P4[:, hd, mt * 128:(mt + 1) * 128],
                                                           C.ident[:]), reads=[('P', hd), 'ident'], writes=[ptn])
                    cp(C, 'dve' if hd % 2 == 0 else 'act', PT4[:, hd], pt_[:, 0:256].rearrange("p (m t) -> p m t", m=2),
                       [ptn], [('PT', hd)])
                for hd in range(4):
                    po, pon = C.nb()
                    for mt in range(2):
                        mm(C, po[:, :], PT4[:, hd, mt, :], vm[:, mt, hd * 512:(hd + 1) * 512], mt == 0, mt == 1,
                           [('PT', hd), 'vm'], [pon])
                    S.op('dve', lambda e: e.reciprocal(out=rs4[:, hd:hd + 1], in_=rs4[:, hd:hd + 1]),
                         reads=[('rs', hd)], writes=[('rs', hd)])
                    act(C, xn[:, hd * 512:(hd + 1) * 512], po[:, :], AF.Copy, [pon, ('rs', hd)], ['xn'], scale=rs4[:, hd:hd + 1])
                transpose_to(C, xn[:], 'xn', KT, mT, 'mT', tsls[j])
            for cb in range(4):
                cs_ = slice(cb * 512, (cb + 1) * 512)
                w, wrs = next_wbig('xo', cb)
                for j in range(4):
                    p, pn = C.nb()
                    for k in range(KT):
                        mm(C, p[:, :], mT[:, k, tsls[j]], w[:, k, :], k == 0, k == KT - 1, ['mT'] + wrs, [pn])
                    tt(C, big[:, j, cs_], big[:, j, cs_], p[:, :], ALU.add, [('big', j), pn], [('big', j)])
            S.dma('sp', gain[:], I['g_ffn'], writes=['gain'])
            x3T = xbuf[1][:].rearrange("p (k t) -> p k t", k=KT)
            for j in range(4):
                t = g * 4 + j
                S.dma('sp', Z['H2'][t], big[:, j, :], reads=[('big', j)], writes=[('H2', t)])
                norm_T(C, big[:, j, :], ('big', j), gain[:], 'gain', xn[:], 'xn', W, x3T, 'x1', slice(0, 128))
                S.dma('sp', Z['XN3'][t], xn[:], reads=['xn'], writes=[('XN3', t)])
                p, pn = C.nb()
                for k in range(KT):
                    mm(C, p[:, 0:36], x3T[:, k, :], wrt[:, k, :], k == 0, k == KT - 1, ['x1', 'wrt'], [pn])
                tt(C, lg[:], p[:, 0:36], brt[:], ALU.add, [pn, 'brt'], ['lg'])
                S.op('dve', lambda e: e.reduce_max(out=sm[:, 0:1], in_=lg[:, 0:4], axis=AX.X), reads=['lg'], writes=['sm'])
                ts(C, ohg[:], lg[:, 0:4], sm[:, 0:1], None, ALU.is_equal, None, ['lg', 'sm'], ['ohg'])
                ts(C, sm[:, 1:2], sm[:, 0:1], -1.0, None, ALU.mult, None, ['sm'], ['sm'])
                act(C, ge[:], lg[:, 0:4], AF.Exp, ['lg', 'sm'], ['ge', 'sm'], bias=sm[:, 1:2], scale=1.0,
                    accum_out=sm[:, 2:3])
                S.op('dve', lambda e: e.reciprocal(out=sm[:, 3:4], in_=sm[:, 2:3]), reads=['sm'], writes=['sm'])
                tt(C, ge[:], ohg[:], jrow[:], ALU.mult, ['ohg', 'jrow'], ['ge'])
                S.op('dve', lambda e: e.reduce_sum(out=sm[:, 4:5], in_=ge[:], axis=AX.X), reads=['ge'], writes=['sm'])
                ts(C, esel[:], lg[:, 4:12], ohg[:, 0:1], None, ALU.mult, None, ['lg', 'ohg'], ['esel'])
                for gj in range(1, 4):
                    stt(C, esel[:], lg[:, 4 + 8 * gj:12 + 8 * gj], ohg[:, gj:gj + 1], esel[:], ALU.mult, ALU.add,
                        ['lg', 'ohg', 'esel'], ['esel'])
                S.op('dve', lambda e: e.max(out=top8[:], in_=esel[:]), reads=['esel'], writes=['top8'])
                S.op('dve', lambda e: e.max_index(out=idx8[:], in_max=top8[:], in_values=esel[:]),
                     reads=['top8', 'esel'], writes=['idx8'])
                cp(C, 'dve', if8[:], idx8[:], ['idx8'], ['if8'])
                tt(C, sm[:, 5:6], top8[:, 1:2], top8[:, 0:1], ALU.subtract, ['top8'], ['sm'])
                act(C, sm[:, 6:7], sm[:, 5:6], AF.Exp, ['sm'], ['sm'])
                ts(C, sm[:, 7:8], sm[:, 6:7], 1.0, None, ALU.add, None, ['sm'], ['sm'])
                S.op('dve', lambda e: e.reciprocal(out=sm[:, 7:8], in_=sm[:, 7:8]), reads=['sm'], writes=['sm'])
                tt(C, RT[:, t, 2:3], sm[:, 3:4], sm[:, 7:8], ALU.mult, ['sm'], ['RT'])
                tt(C, RT[:, t, 3:4], RT[:, t, 2:3], sm[:, 6:7], ALU.mult, ['RT', 'sm'], ['RT'])
                stt(C, RT[:, t, 0:1], sm[:, 4:5], 8.0, if8[:, 0:1], ALU.mult, ALU.add, ['sm', 'if8'], ['RT'])
                stt(C, RT[:, t, 1:2], sm[:, 4:5], 8.0, if8[:, 1:2], ALU.mult, ALU.add, ['sm', 'if8'], ['RT'])


def stage_moe(C):
    S = C.S
    I = C.I
    Z = C.Z
    RT = C.RT
    nc = C.nc
    with ExitStack() as es1:
        def sbp(name, shape, dt):
            return es1.enter_context(C.nc.sbuf_tensor("m_" + name, shape, dt))
        idx = sbp("idx", [128, NT_OWN, 2], I32)
        bei = sbp("bei", [1, 64], I32)
        with ExitStack() as es2:
            def sbt(name, shape, dt):
                return es2.enter_context(C.nc.sbuf_tensor("md_" + name, shape, dt))
            iota = sbt("iota", [128, 32], F32)
            brow = sbt("brow", [32, 64], F32)
            M = sbt("M", [128, NT_OWN, 32], F32)
            cf = sbt("cf", [128, 32], F32)
            ci = sbt("ci", [128, 32], I32)
            padf = sbt("padf", [128, 32], F32)
            pend = sbt("pend", [128, 32], F32)
            pst = sbt("pst", [128, 32], F32)
            dst = sbt("dst", [128, 32], F32)
            oh = sbt("oh", [128, 32], F32)
            cT = sbt("cT", [32, 1], F32)
            cTi = sbt("cTi", [32, 1], I32)
            pT = sbt("pT", [32, 1], F32)
            cmp_ = sbt("cmp", [32, 64], F32)
            bef = sbt("bef", [1, 64], F32)
            neq = sbt("neq", [1, 64], F32)
            ldf = sbt("ldf", [1, 64], F32)
            xr = [sbt("xr%d" % i, [128, D], F32) for i in range(2)]
            S.dma('sp', iota[:], I['c_iota32'], writes=['iota'])
            S.dma('sp', brow[:], I['c_brow'], writes=['brow'])
            for t in range(NT_OWN):
                ts(C, M[:, t, :], iota[:], RT[:, t, 0:1], None, ALU.is_equal, None, ['iota', 'RT'], [('M', t)])
                stt(C, M[:, t, :], iota[:], RT[:, t, 1:2], M[:, t, :], ALU.is_equal, ALU.add, ['iota', 'RT', ('M', t)], [('M', t)])
            p, pn = C.nb()
            for t in range(NT_OWN):
                mm(C, p[:, 0:32], C.ones[:], M[:, t, :], t == 0, t == NT_OWN - 1, ['ones', ('M', t)], [pn])
            ts(C, cf[:], p[:, 0:32], 127.0, None, ALU.add, None, [pn], ['cf'])
            cp(C, 'dve', ci[:], cf[:], ['cf'], ['ci'])
            ts(C, ci[:], ci[:], 7, None, ALU.arith_shift_right, None, ['ci'], ['ci'])
            ts(C, ci[:], ci[:], 7, None, ALU.logical_shift_left, None, ['ci'], ['ci'])
            cp(C, 'dve', padf[:], ci[:], ['ci'], ['padf'])
            S.op('dve', lambda e: e.tensor_tensor_scan(out=pend[:], data0=C.ones[:, 0:32], data1=padf[:], initial=0.0,
                                                      op0=ALU.mult, op1=ALU.add), reads=['ones', 'padf'], writes=['pend'])
            tt(C, pst[:], pend[:], padf[:], ALU.subtract, ['pend', 'padf'], ['pst'])
            for t in range(NT_OWN):
                p, pn = C.nb()
                for j in range(t):
                    mm(C, p[:, 0:32], C.ones[:], M[:, j, :], j == 0, False, ['ones', ('M', j)], [pn])
                mm(C, p[:, 0:32], C.ustr[:, 1, :], M[:, t, :], t == 0, True, ['ustr', ('M', t)], [pn])
                tt(C, dst[:], p[:, 0:32], pst[:], ALU.add, [pn, 'pst'], ['dst'])
                for a_i in range(2):
                    ts(C, oh[:], iota[:], RT[:, t, a_i:a_i + 1], None, ALU.is_equal, None, ['iota', 'RT'], ['oh'])
                    tt(C, oh[:], oh[:], dst[:], ALU.mult, ['oh', 'dst'], ['oh'])
                    S.op('dve', lambda e: e.reduce_sum(out=RT[:, t, 4 + a_i:5 + a_i], in_=oh[:], axis=AX.X),
                         reads=['oh'], writes=['RT'])
            cp(C, 'dve', idx[:], RT[:, :, 4:6], ['RT'], ['idx'])
            for t in range(NT_OWN):
                S.dma('sp', Z['RT'][t], RT[:, t, :], reads=['RT'], writes=[('RTd', t)])
            p, pn = C.nb()
            for t in range(NT_OWN):
                mm(C, p[0:32, 0:1], M[:, t, :], C.ones[:, 0:1], t == 0, t == NT_OWN - 1, [('M', t), 'ones'], [pn])
            ts(C, cT[:], p[0:32, 0:1], 127.0, None, ALU.add, None, [pn], ['cT'])
            cp(C, 'dve', cTi[:], cT[:], ['cT'], ['cTi'])
            ts(C, cTi[:], cTi[:], 7, None, ALU.arith_shift_right, None, ['cTi'], ['cTi'])
            ts(C, cTi[:], cTi[:], 7, None, ALU.logical_shift_left, None, ['cTi'], ['cTi'])
            cp(C, 'dve', cT[:], cTi[:], ['cTi'], ['cT'])
            p, pn = C.nb()
            mm(C, p[0:32, 0:1], C.mincl[0:32, 0, 0:32], cT[:], True, True, ['mincl', 'cT'], [pn])
            cp(C, 'dve', pT[:], p[0:32, 0:1], [pn], ['pT'])
            ts(C, cmp_[:], brow[:], pT[:, 0:1], None, ALU.is_ge, None, ['brow', 'pT'], ['cmp'])
            p, pn = C.nb()
            mm(C, p[0:1, 0:64], C.ones[0:32, 0:1], cmp_[:], True, True, ['ones', 'cmp'], [pn])
            cp(C, 'dve', bef[:], p[0:1, 0:64], [pn], ['bef'])
            S.op('dve', lambda e: e.memset(neq[:], 1.0), writes=['neq'])
            tt(C, neq[0:1, 1:64], bef[0:1, 1:64], bef[0:1, 0:63], ALU.not_equal, ['bef'], ['neq'])
            ts(C, ldf[:], bef[:], -40.0, None, ALU.add, None, ['bef'], ['ldf'])
            tt(C, ldf[:], ldf[:], neq[:], ALU.mult, ['ldf', 'neq'], ['ldf'])
            ts(C, ldf[:], ldf[:], 40.0, None, ALU.add, None, ['ldf'], ['ldf'])
            cp(C, 'dve', bei[:], ldf[:], ['ldf'], ['bei'])
            S.dma('sp', Z['BE'], bef[:], reads=['bef'], writes=['BEd'])
            for t in range(NT_OWN):
                xb, xbr = xr[t % 2], 'xr%d' % (t % 2)
                S.dma('sp', xb[:], Z['XN3'][t], writes=[xbr])
                for a_i in range(2):
                    S.dma_indirect('pool', out=Z['XR'], out_offset=bass.IndirectOffsetOnAxis(ap=idx[:, t, a_i:a_i + 1], axis=0),
                                   in_=xb[:], in_offset=None, reads=[xbr, 'idx'] + [('XRz', b) for b in range(NBLK)],
                                   writes=[('XR', b) for b in range(NBLK)])
            S.barrier()
        with ExitStack() as es2:
            def sbt(name, shape, dt):
                return es2.enter_context(C.nc.sbuf_tensor("me_" + name, shape, dt))
            wgs = sbt("wgs", [128, KT, 512], F32)
            wus = sbt("wus", [128, KT, 512], F32)
            wds = sbt("wds", [128, 4, D], F32)
            wg = sbt("wg", [128, KT, 512], BF16)
            wu = sbt("wu", [128, KT, 512], BF16)
            wd = sbt("wd", [128, 4, D], BF16)
            x2 = [sbt("x%d" % i, [128, D], F32) for i in range(2)]
            y2 = [sbt("y%d" % i, [128, D], F32) for i in range(2)]
            xrT = sbt("xrT", [128, KT, 128], BF16)
            a_ = sbt("a", [128, 512], F32)
            aT = sbt("aT", [128, 4, 128], BF16)

            def issue_loads(b):
                q = ('pool', 'sp', 'act')[b % 3] if b < 60 else ('sp', 'act')[b % 2]
                S.wait_for(q, ['bei'])
                ev = S.eng[q].value_load(bei[0:1, b:b + 1])
                for (wt, wn, src) in ((wgs, 'wgs', 'w_gate'), (wus, 'wus', 'w_up'), (wds, 'wds', 'w_down')):
                    S.dma(q, wt[:], I[src].rearrange("e (p k) f -> e p k f", p=128)[bass.ds(ev, 1)].rearrange("a p k f -> (a p) k f"),
                          reads=['bei'], writes=[wn], bounds_check="skip_entire_dma")
                S.eng[q].free_register(ev.val)

            def issue_x(b):
                S.dma('sp', x2[b % 2][:], Z['XR'][b * 128:(b + 1) * 128, :], reads=[('XR', b)], writes=['x%d' % (b % 2)])

            issue_loads(0)
            issue_x(0)
            for b in range(NBLK):
                xb, xbr = x2[b % 2], 'x%d' % (b % 2)
                yb, ybr = y2[b % 2], 'y%d' % (b % 2)
                cp(C, 'dve', wg[:, 0:8, :], wgs[:, 0:8, :], ['wgs'], ['wg_lo'])
                cp(C, 'act', wg[:, 8:16, :], wgs[:, 8:16, :], ['wgs'], ['wg_hi'])
                cp(C, 'dve', wu[:, 0:8, :], wus[:, 0:8, :], ['wus'], ['wu_lo'])
                cp(C, 'act', wu[:, 8:16, :], wus[:, 8:16, :], ['wus'], ['wu_hi'])
                cp(C, 'dve', wd[:, 0:2, :], wds[:, 0:2, :], ['wds'], ['wd0'])
                cp(C, 'act', wd[:, 2:4, :], wds[:, 2:4, :], ['wds'], ['wd1'])
                if b + 1 < NBLK:
                    issue_loads(b + 1)
                    issue_x(b + 1)
                transpose_to(C, xb[:], xbr, KT, xrT, 'xrT', slice(0, 128), interleave=True)
                hg, hgn = C.nb()
                for k in range(KT):
                    mm(C, hg[:, :], xrT[:, k, :], wg[:, k, :], k == 0, k == KT - 1, ['xrT', 'wg_lo' if k < 8 else 'wg_hi'], [hgn])
                hu, hun = C.nb()
                for k in range(KT):
                    mm(C, hu[:, :], xrT[:, k, :], wu[:, k, :], k == 0, k == KT - 1, ['xrT', 'wu_lo' if k < 8 else 'wu_hi'], [hun])
                act(C, a_[:], hg[:, :], AF.Silu, [hgn], ['a'])
                tt(C, a_[:], a_[:], hu[:, :], ALU.mult, ['a', hun], ['a'])
                transpose_to(C, a_[:], 'a', 4, aT, 'aT', slice(0, 128), interleave=True)
                for cb in range(4):
                    y, yn_ = C.nb()
                    for k in range(4):
                        mm(C, y[:, :], aT[:, k, :], wd[:, k, cb * 512:(cb + 1) * 512], k == 0, k == 3,
                           ['aT', 'wd0' if k < 2 else 'wd1'], [yn_])
                    cp(C, 'act' if cb % 2 == 0 else 'dve', yb[:, cb * 512:(cb + 1) * 512], y[:, :], [yn_], [ybr])
                S.dma('sp', Z['YR'][b * 128:(b + 1) * 128, :], yb[:], reads=[ybr], writes=[('YR', b)])
            S.barrier()
        with ExitStack() as es2:
            def sbt(name, shape, dt):
                return es2.enter_context(C.nc.sbuf_tensor("mc_" + name, shape, dt))
            W = {}
            W['junk'] = sbt("junk", [128, D], BF16)
            W['ss'] = sbt("ss", [128, 1], F32)
            W['rstd'] = sbt("rstd", [128, 1], F32)
            gain = sbt("gain", [128, D], F32)
            NBUF = 3
            rb = [[sbt("r%d%d" % (a_i, i), [128, D], F32) for i in range(NBUF)] for a_i in range(2)]
            hb = [sbt("h%d" % i, [128, D], F32) for i in range(NBUF)]
            S.dma('sp', gain[:], I['g_final'], writes=['gain'])
            for t in range(NT_OWN):
                i = t % NBUF
                r0, r0n = rb[0][i], 'r0%d' % i
                r1, r1n = rb[1][i], 'r1%d' % i
                h2, h2n = hb[i], 'h%d' % i
                S.dma_indirect('pool', out=r0[:], out_offset=None, in_=Z['YR'],
                               in_offset=bass.IndirectOffsetOnAxis(ap=idx[:, t, 0:1], axis=0),
                               reads=['idx'], writes=[r0n])
                S.dma_indirect('pool', out=r1[:], out_offset=None, in_=Z['YR'],
                               in_offset=bass.IndirectOffsetOnAxis(ap=idx[:, t, 1:2], axis=0),
                               reads=['idx'], writes=[r1n])
                S.dma('pool', h2[:], Z['H2'][t], writes=[h2n])
                stt(C, h2[:], r0[:], RT[:, t, 2:3], h2[:], ALU.mult, ALU.add, [r0n, 'RT', h2n], [h2n])
                stt(C, h2[:], r1[:], RT[:, t, 3:4], h2[:], ALU.mult, ALU.add, [r1n, 'RT', h2n], [h2n])
                rms_rstd(C, h2[:], h2n, D, W['junk'][:], 'junk', W['ss'][:], W['rstd'][:], 'fn_')
                stt(C, h2[:], h2[:], W['rstd'][:, 0:1], gain[:], ALU.mult, ALU.mult, [h2n, 'fn_rstd', 'gain'], [h2n])
                S.dma('sp', C.out[t * 128:(t + 1) * 128, :], h2[:], reads=[h2n], writes=['out'])
```
